# Optimizing a Trainium2 kernel written in Bass

```python
import math
import jax, jax.numpy as jnp
from jax import lax
import numpy as np

D_MODEL = 1024
BATCH = 8
SEQ = 2048
DEPTH = 4

POOL_WINDOWS = (2, 4, 8, 16)
POOL_WIDTH = 512
POOL_GROUP = POOL_WIDTH // len(POOL_WINDOWS)
ATT_HEADS = 8
HEAD_DIM = 64
ATT_WIDTH = ATT_HEADS * HEAD_DIM
MOBA_BLOCK = 256
MOBA_TOPK = 3
Q_CHUNK = 16
SSM_WIDTH = 512
SSM_GROUP = 16
SSM_GROUPS = SSM_WIDTH // SSM_GROUP
SSM_STATE = 64
DT_MIN = 1e-3
DT_MAX = 1e-1
N_BRANCH = 3
BRANCH_WIDTH = 512
IN_WIDTH = POOL_WIDTH + 3 * ATT_WIDTH + SSM_WIDTH + N_BRANCH * D_MODEL
D_FF = 2816
N_EXPERTS = 8
TOP_K = 2
N_DENSE = (DEPTH + 1) // 2
N_MOE = DEPTH // 2
DN_ALPHA = (2 * DEPTH) ** 0.25
DN_BETA = (8 * DEPTH) ** -0.25
LN_EPS = 1e-5

kernel_name = "hybrid_gated_pool_moba_s5_moe"


def layer_norm(x, g, b):
    xf = x.astype(jnp.float32)
    mu = jnp.mean(xf, axis=-1, keepdims=True)
    var = jnp.mean(jnp.square(xf - mu), axis=-1, keepdims=True)
    y = (xf - mu) * lax.rsqrt(var + LN_EPS) * g.astype(jnp.float32) + b.astype(jnp.float32)
    return y.astype(x.dtype)


def pool_mixer(u, w_pool, pool_scale):
    S_ = u.shape[1]
    uf = u.astype(jnp.float32)
    cs = jnp.cumsum(uf, axis=1)
    cs = jnp.concatenate([jnp.zeros_like(cs[:, :1]), cs], axis=1)
    t = jnp.arange(S_)
    outs = []
    for gi, w in enumerate(POOL_WINDOWS):
        c = cs[..., gi * POOL_GROUP:(gi + 1) * POOL_GROUP]
        start = jnp.maximum(t + 1 - w, 0)
        count = (t + 1 - start).astype(jnp.float32)[:, None]
        mean = (c[:, 1:] - c[:, start]) / count
        outs.append(mean - uf[..., gi * POOL_GROUP:(gi + 1) * POOL_GROUP])
    z = jnp.stack(outs, axis=2)
    z = jnp.einsum('bsgc,gcd->bsgd', z, w_pool.astype(jnp.float32))
    y = z.reshape(u.shape) * pool_scale.astype(jnp.float32)
    return y.astype(u.dtype)


def moba_attention(q, k, v):
    B_, S_, H, dh = q.shape
    nb = -(-S_ // MOBA_BLOCK)
    pad = nb * MOBA_BLOCK - S_
    q = q.transpose(0, 2, 1, 3) * (dh ** -0.5)
    k = jnp.pad(k.transpose(0, 2, 1, 3), ((0, 0), (0, 0), (0, pad), (0, 0)))
    v = jnp.pad(v.transpose(0, 2, 1, 3), ((0, 0), (0, 0), (0, pad), (0, 0)))
    kb = k.reshape(B_, H, nb, MOBA_BLOCK, dh)
    vb = v.reshape(B_, H, nb, MOBA_BLOCK, dh)
    k_mean = jnp.mean(kb.astype(jnp.float32), axis=3)
    t = jnp.arange(S_)
    q_blk = t // MOBA_BLOCK
    gate = jnp.einsum('bhtd,bhnd->bhtn', q.astype(jnp.float32), k_mean)
    past = jnp.arange(nb)[None, :] < q_blk[:, None]
    gate = jnp.where(past, gate, -jnp.inf)
    k_sel = min(MOBA_TOPK, nb)
    _, sel = lax.top_k(gate, k_sel)
    sel_valid = jnp.arange(k_sel)[None, :] < q_blk[:, None]
    slopes = jnp.exp2(-8.0 * jnp.arange(1, H + 1, dtype=jnp.float32) / H)
    b_idx = jnp.arange(B_)[:, None, None, None]
    h_idx = jnp.arange(H)[None, :, None, None]

    def chunk(c):
        t0 = c * Q_CHUNK
        qc = lax.dynamic_slice_in_dim(q, t0, Q_CHUNK, axis=2)
        sc = lax.dynamic_slice_in_dim(sel, t0, Q_CHUNK, axis=2)
        vc = lax.dynamic_slice_in_dim(sel_valid, t0, Q_CHUNK, axis=0)
        tq = t0 + jnp.arange(Q_CHUNK)
        kg = kb[b_idx, h_idx, sc]
        vg = vb[b_idx, h_idx, sc]
        s_pos = sc[..., None] * MOBA_BLOCK + jnp.arange(MOBA_BLOCK)
        dist = (tq[None, None, :, None, None] - s_pos).astype(jnp.float32)
        s_past = jnp.einsum('bhqd,bhqnkd->bhqnk', qc, kg).astype(jnp.float32)
        s_past = s_past - slopes[None, :, None, None, None] * dist
        s_past = jnp.where(vc[None, None, :, :, None], s_past, -jnp.inf)
        s_past = s_past.reshape(B_, H, Q_CHUNK, k_sel * MOBA_BLOCK)
        j0 = (t0 // MOBA_BLOCK) * MOBA_BLOCK
        ko = lax.dynamic_slice_in_dim(k, j0, MOBA_BLOCK, axis=2)
        vo = lax.dynamic_slice_in_dim(v, j0, MOBA_BLOCK, axis=2)
        so_pos = j0 + jnp.arange(MOBA_BLOCK)
        dist_o = (tq[:, None] - so_pos[None, :]).astype(jnp.float32)
        s_own = jnp.einsum('bhqd,bhkd->bhqk', qc, ko).astype(jnp.float32)
        s_own = s_own - slopes[None, :, None, None] * dist_o
        s_own = jnp.where((so_pos[None, :] <= tq[:, None]), s_own, -jnp.inf)
        p = jax.nn.softmax(jnp.concatenate([s_past, s_own], axis=-1), axis=-1)
        p_past = p[..., :k_sel * MOBA_BLOCK].reshape(B_, H, Q_CHUNK, k_sel, MOBA_BLOCK).astype(v.dtype)
        p_own = p[..., k_sel * MOBA_BLOCK:].astype(v.dtype)
        out = jnp.einsum('bhqnk,bhqnkd->bhqd', p_past, vg) + jnp.einsum('bhqk,bhkd->bhqd', p_own, vo)
        return out.astype(q.dtype)

    outs = lax.map(chunk, jnp.arange(S_ // Q_CHUNK))
    return outs.transpose(1, 0, 3, 2, 4).reshape(B_, S_, H, dh)


def s5_mixer(u, a_re, a_im, log_dt, b_re, b_im, c_re, c_im, d_skip, w_glu, b_glu):
    B_, S_, _ = u.shape
    f32 = jnp.float32
    ug = u.astype(f32).reshape(B_, S_, SSM_GROUPS, SSM_GROUP)
    dt = jnp.exp(log_dt.astype(f32))[:, None]
    ar, ai = a_re.astype(f32), a_im.astype(f32)
    mag = jnp.exp(ar * dt)
    lr, li = mag * jnp.cos(ai * dt), mag * jnp.sin(ai * dt)
    den = ar * ar + ai * ai
    nr = lr - 1.0
    fr = (nr * ar + li * ai) / den
    fi = (li * ar - nr * ai) / den
    br, bi = b_re.astype(f32), b_im.astype(f32)
    bbr = fr[..., None] * br - fi[..., None] * bi
    bbi = fr[..., None] * bi + fi[..., None] * br
    xr = jnp.einsum('bsgp,gnp->bsgn', ug, bbr)
    xi = jnp.einsum('bsgp,gnp->bsgn', ug, bbi)
    lr_t = jnp.broadcast_to(lr, xr.shape)
    li_t = jnp.broadcast_to(li, xr.shape)

    def combine(e1, e2):
        a1r, a1i, h1r, h1i = e1
        a2r, a2i, h2r, h2i = e2
        return (a2r * a1r - a2i * a1i, a2r * a1i + a2i * a1r,
                a2r * h1r - a2i * h1i + h2r, a2r * h1i + a2i * h1r + h2i)

    _, _, hr, hi = lax.associative_scan(combine, (lr_t, li_t, xr, xi), axis=1)
    y = (jnp.einsum('bsgn,gpn->bsgp', hr, c_re.astype(f32))
         - jnp.einsum('bsgn,gpn->bsgp', hi, c_im.astype(f32))
         + d_skip.astype(f32).reshape(SSM_GROUPS, SSM_GROUP) * ug)
    y = jax.nn.gelu(y.reshape(B_, S_, SSM_WIDTH))
    z = y @ w_glu.astype(f32) + b_glu.astype(f32)
    out = z[..., :SSM_WIDTH] * jax.nn.sigmoid(z[..., SSM_WIDTH:])
    return out.astype(u.dtype)


def mixer_sublayer(x, w_in, b_in, w_pool, pool_scale, a_re, a_im, log_dt, b_re, b_im,
                   c_re, c_im, d_skip, w_glu, b_glu, w_branch, w_out):
    B_, S_, D = x.shape
    z = x @ w_in + b_in
    cuts = [POOL_WIDTH, POOL_WIDTH + ATT_WIDTH, POOL_WIDTH + 2 * ATT_WIDTH,
            POOL_WIDTH + 3 * ATT_WIDTH, POOL_WIDTH + 3 * ATT_WIDTH + SSM_WIDTH]
    u_a, q, k, v, u_c, g = jnp.split(z, cuts, axis=-1)
    y_a = pool_mixer(u_a, w_pool, pool_scale)
    hs = (B_, S_, ATT_HEADS, HEAD_DIM)
    y_b = moba_attention(q.reshape(hs), k.reshape(hs), v.reshape(hs)).reshape(B_, S_, ATT_WIDTH)
    y_c = s5_mixer(u_c, a_re, a_im, log_dt, b_re, b_im, c_re, c_im, d_skip, w_glu, b_glu)
    ys = jnp.stack([y_a, y_b, y_c], axis=2)
    proj = jnp.einsum('bsnc,ncd->bsnd', ys, w_branch)
    gates = jax.nn.sigmoid(g.reshape(B_, S_, N_BRANCH, D))
    merged = jnp.sum(gates * proj, axis=2)
    return merged @ w_out


def swiglu(x, w1, w3, w2):
    return (jax.nn.silu(x @ w1) * (x @ w3)) @ w2


def moe_swiglu(x, router, router_b, w1, w3, w2):
    logits = (x @ router + router_b).astype(jnp.float32)
    top_val, top_idx = lax.top_k(logits, TOP_K)
    top_w = jax.nn.softmax(top_val, axis=-1)
    gate = jnp.sum(jax.nn.one_hot(top_idx, N_EXPERTS, dtype=jnp.float32) * top_w[..., None], axis=-2)
    y = jnp.zeros_like(x)
    for e in range(N_EXPERTS):
        y = y + gate[..., e:e + 1].astype(x.dtype) * swiglu(x, w1[e], w3[e], w2[e])
    return y


def setup_inputs(seed: int = 0) -> dict:
    key = jax.random.key(seed)
    ks = iter(jax.random.split(key, 40))
    f32 = jnp.float32

    def nrm(shape, scale):
        return jax.random.normal(next(ks), shape, f32) * scale

    G, N, P = SSM_GROUPS, SSM_STATE, SSM_GROUP
    n_idx = jnp.arange(N, dtype=f32)[None, None, :]
    return {
        "x": nrm((BATCH, SEQ, D_MODEL), 1.0),
        "ln1_g": 1.0 + nrm((DEPTH, D_MODEL), 0.02),
        "ln1_b": nrm((DEPTH, D_MODEL), 0.02),
        "w_in": nrm((DEPTH, D_MODEL, IN_WIDTH), D_MODEL ** -0.5),
        "b_in": nrm((DEPTH, IN_WIDTH), 0.01),
        "w_pool": nrm((DEPTH, len(POOL_WINDOWS), POOL_GROUP, POOL_GROUP), POOL_GROUP ** -0.5),
        "pool_scale": 1.0 + nrm((DEPTH, POOL_WIDTH), 0.02),
        "ssm_a_re": -0.5 + nrm((DEPTH, G, N), 0.01),
        "ssm_a_im": math.pi * n_idx + nrm((DEPTH, G, N), 0.01),
        "ssm_log_dt": jax.random.uniform(next(ks), (DEPTH, G), f32, math.log(DT_MIN), math.log(DT_MAX)),
        "ssm_b_re": nrm((DEPTH, G, N, P), (2 * P) ** -0.5),
        "ssm_b_im": nrm((DEPTH, G, N, P), (2 * P) ** -0.5),
        "ssm_c_re": nrm((DEPTH, G, P, N), (2 * N) ** -0.5),
        "ssm_c_im": nrm((DEPTH, G, P, N), (2 * N) ** -0.5),
        "ssm_d": nrm((DEPTH, SSM_WIDTH), 1.0),
        "w_glu": nrm((DEPTH, SSM_WIDTH, 2 * SSM_WIDTH), SSM_WIDTH ** -0.5),
        "b_glu": nrm((DEPTH, 2 * SSM_WIDTH), 0.01),
        "w_branch": nrm((DEPTH, N_BRANCH, BRANCH_WIDTH, D_MODEL), BRANCH_WIDTH ** -0.5),
        "w_out": nrm((DEPTH, D_MODEL, D_MODEL), D_MODEL ** -0.5 * DN_BETA),
        "ln2_g": 1.0 + nrm((DEPTH, D_MODEL), 0.02),
        "ln2_b": nrm((DEPTH, D_MODEL), 0.02),
        "ffn_w1": nrm((N_DENSE, D_MODEL, D_FF), D_MODEL ** -0.5),
        "ffn_w3": nrm((N_DENSE, D_MODEL, D_FF), D_MODEL ** -0.5),
        "ffn_w2": nrm((N_DENSE, D_FF, D_MODEL), D_FF ** -0.5 * DN_BETA),
        "moe_router": nrm((N_MOE, D_MODEL, N_EXPERTS), D_MODEL ** -0.5),
        "moe_router_b": nrm((N_MOE, N_EXPERTS), 0.01),
        "moe_w1": nrm((N_MOE, N_EXPERTS, D_MODEL, D_FF), D_MODEL ** -0.5),
        "moe_w3": nrm((N_MOE, N_EXPERTS, D_MODEL, D_FF), D_MODEL ** -0.5),
        "moe_w2": nrm((N_MOE, N_EXPERTS, D_FF, D_MODEL), D_FF ** -0.5 * DN_BETA),
    }


def reference(x, ln1_g, ln1_b, w_in, b_in, w_pool, pool_scale, ssm_a_re, ssm_a_im, ssm_log_dt,
              ssm_b_re, ssm_b_im, ssm_c_re, ssm_c_im, ssm_d, w_glu, b_glu, w_branch, w_out,
              ln2_g, ln2_b, ffn_w1, ffn_w3, ffn_w2, moe_router, moe_router_b, moe_w1, moe_w3, moe_w2):
    h = x
    for i in range(DEPTH):
        mix = mixer_sublayer(h, w_in[i], b_in[i], w_pool[i], pool_scale[i], ssm_a_re[i], ssm_a_im[i],
                             ssm_log_dt[i], ssm_b_re[i], ssm_b_im[i], ssm_c_re[i], ssm_c_im[i], ssm_d[i],
                             w_glu[i], b_glu[i], w_branch[i], w_out[i])
        h = layer_norm(DN_ALPHA * h + mix, ln1_g[i], ln1_b[i])
        j = i // 2
        if i % 2 == 0:
            f = swiglu(h, ffn_w1[j], ffn_w3[j], ffn_w2[j])
        else:
            f = moe_swiglu(h, moe_router[j], moe_router_b[j], moe_w1[j], moe_w3[j], moe_w2[j])
        h = layer_norm(DN_ALPHA * h + f, ln2_g[i], ln2_b[i])
    return h
```

```python
import concourse.bass as bass
import concourse.mybir as mybir

F32 = mybir.dt.float32
BF16 = mybir.dt.bfloat16
ALU = mybir.AluOpType
AF = mybir.ActivationFunctionType
AX = mybir.AxisListType

EPOCH = 30000


class Prog:
    def __init__(self, nc, es):
        self.nc = nc
        self.es = es
        self.eng = {"pe": nc.tensor, "act": nc.scalar, "dve": nc.vector,
                    "pool": nc.gpsimd, "sp": nc.sync}
        self.cur = {}
        self.waited = {e: {} for e in self.eng}
        self.last_w = {}
        self.rd = {}
        self.dsem = {}
        self.nsem = 0
        self.nins = {e: 0 for e in self.eng}
        self.all_events = []

    def _newsem(self, name):
        self.nsem += 1
        return self.es.enter_context(self.nc.semaphore(f"{name}_{self.nsem}"))

    def _engsem(self, e):
        c = self.cur.get(e)
        if c is None or c[1] >= EPOCH:
            c = [self._newsem("s" + e), 0]
            self.cur[e] = c
        return c

    def _deps(self, reads, writes):
        ev = []
        for k in reads:
            w = self.last_w.get(k)
            if w is not None:
                ev.append(w)
        for k in writes:
            w = self.last_w.get(k)
            if w is not None:
                ev.append(w)
            ev.extend(self.rd.get(k, ()))
        return ev

    def _emit_waits(self, e, evs, skip_self_sem=None):
        best = {}
        for (s, v) in evs:
            if skip_self_sem is not None and s.num == skip_self_sem.num:
                continue
            if v > best.get(s.num, (None, 0))[1]:
                best[s.num] = (s, v)
        wd = self.waited[e]
        for num, (s, v) in best.items():
            if wd.get(num, 0) >= v:
                continue
            self.eng[e].wait_ge(s, v)
            wd[num] = v

    def _record(self, ev, reads, writes):
        for k in writes:
            self.last_w[k] = ev
            self.rd[k] = []
        for k in reads:
            if k in writes:
                continue
            self.rd.setdefault(k, []).append(ev)

    def op(self, e, fn, reads=(), writes=(), pe_chain=False):
        psr = [k for k in reads if isinstance(k, tuple) and k[0] == "ps"]
        if psr:
            reads = [k for k in reads if k not in psr]
            writes = list(writes) + [k for k in psr if k not in writes]
        evs = self._deps(reads, writes)
        c = self._engsem(e)
        self._emit_waits(e, evs, skip_self_sem=c[0] if (e == "pe" and pe_chain) else None)
        ins = fn(self.eng[e])
        c[1] += 1
        ins.then_inc(c[0], 1)
        ev = (c[0], c[1])
        self._record(ev, reads, writes)
        self.nins[e] += 1
        return ev

    def dma(self, e, out, in_, reads=(), writes=(), semkey=None, **kw):
        evs = self._deps(reads, writes)
        self._emit_waits(e, evs)
        d = self.dsem.get(semkey)
        if d is None:
            d = [self._newsem("d"), 0]
            self.dsem[semkey] = d
        ins = self.eng[e].dma_start(out=out, in_=in_, **kw)
        d[1] += 16
        assert d[1] < 2 * EPOCH, f"dma sem overflow {semkey}"
        ins.then_inc(d[0], 16)
        ev = (d[0], d[1])
        self._record(ev, reads, writes)
        self.nins[e] += 1
        return ev

    def wait_all(self, e):
        evs = []
        for k, c in self.cur.items():
            if c[1] > 0:
                evs.append((c[0], c[1]))
        for k, d in self.dsem.items():
            if d[1] > 0:
                evs.append((d[0], d[1]))
        for k, w in self.last_w.items():
            evs.append(w)
        for k, r in self.rd.items():
            evs.extend(r)
        self._emit_waits(e, evs)

    def barrier(self):
        for e in self.eng:
            self.wait_all(e)
        self.last_w = {}
        self.rd = {}


import numpy as np
from contextlib import ExitStack
import concourse.bass as bass
import concourse.mybir as mybir
from concourse.bass_utils import run_bass_kernel_spmd

T = 2048
D = 1024
NT = 16
INW = 5632
DFF = 2816
NF = 22
ALPHA = 8 ** 0.25
EPS = 1e-5
NEGM = -240000.0
MAGIC = 12582912.0
TWO_PI_LO = 6.283185
PI_LO = 3.1415925


def host_consts():
    c = {}
    pw = np.zeros((4, 3, 128, 128), np.float32)
    for gi, w in enumerate((2, 4, 8, 16)):
        A = np.zeros((256, 256), np.float32)
        for t in range(256):
            st = max(t + 1 - w, 0)
            A[t, st:t + 1] = 1.0 / (t + 1 - st)
            A[t, t] -= 1.0
        pw[gi, 0] = A[0:128, 0:128].T
        pw[gi, 1] = A[128:256, 128:256].T
        pw[gi, 2] = A[128:256, 0:128].T
    c["c_pw"] = np.ascontiguousarray(pw.transpose(2, 0, 1, 3)).reshape(128, 4 * 3 * 128)
    ind = np.zeros((128, 64, 128), np.float32)
    for r in range(64):
        ind[r, r, :] = 1.0
    c["c_ind"] = ind.reshape(128, 64 * 128)
    slopes = 2.0 ** (-8.0 * np.arange(1, 9) / 8)
    eb = np.zeros((128, 8, 17), np.float32)
    ki = np.arange(128)
    for h in range(8):
        for m in range(17):
            eb[:, h, m] = slopes[h] * (ki - 128 * (m - 1) - 128)
    c["c_expb"] = eb.reshape(128, 136)
    cb = np.zeros((128, 128), np.float32)
    for k in range(128):
        cb[k, :k] = NEGM
    c["c_cbtri"] = cb
    nq = np.zeros((128, 8, 8, 8), np.float32)
    for qb in range(8):
        nq[:, qb, :, qb:] = -1e30
    c["c_negq"] = nq.reshape(128, 8 * 64)
    c["c_ident"] = np.eye(128, dtype=np.float32)
    c["c_iota"] = np.arange(T, dtype=np.float32).reshape(1, T)
    sel8 = np.zeros((8, 8, 128), np.float32)
    for e in range(8):
        sel8[e, e, :] = 1.0
    c["c_sel8"] = sel8.reshape(8, 8 * 128)
    return c


def host_layout(inp):
    o = {}
    L = 4
    o["w_in"] = inp["w_in"]
    o["b_in_fm"] = np.ascontiguousarray(inp["b_in"].reshape(L, 44, 128).transpose(0, 2, 1))
    o["b_in"] = inp["b_in"]
    o["w_pool"] = np.ascontiguousarray(inp["w_pool"].transpose(0, 2, 1, 3)).reshape(L, 128, 512)
    o["pool_scale"] = np.ascontiguousarray(inp["pool_scale"].reshape(L, 4, 128).transpose(0, 2, 1))

    def st_layout(a):
        return np.ascontiguousarray(a.reshape(L, 16, 2, 64).transpose(0, 2, 3, 1)).reshape(L, 128, 16)
    o["a_re"] = st_layout(inp["ssm_a_re"])
    o["a_im"] = st_layout(inp["ssm_a_im"])
    ld = np.repeat(inp["ssm_log_dt"].reshape(L, 16, 2, 1), 64, axis=3)
    o["log_dt"] = np.ascontiguousarray(ld.transpose(0, 2, 3, 1)).reshape(L, 128, 16)

    def b_layout(b):
        out = np.zeros((L, 128, 16, 128), np.float32)
        for g in range(32):
            st, half, gl = g // 2, g % 2, g % 8
            out[:, gl * 16:(gl + 1) * 16, st, half * 64:(half + 1) * 64] = b[:, g].transpose(0, 2, 1)
        return out.reshape(L, 128, 16 * 128)

    def c_layout(cm):
        out = np.zeros((L, 128, 16, 128), np.float32)
        for g in range(32):
            st, half, gl = g // 2, g % 2, g % 8
            out[:, half * 64:(half + 1) * 64, st, gl * 16:(gl + 1) * 16] = cm[:, g].transpose(0, 2, 1)
        return out.reshape(L, 128, 16 * 128)
    o["b_re"] = b_layout(inp["ssm_b_re"])
    o["b_im"] = b_layout(inp["ssm_b_im"])
    o["c_re"] = c_layout(inp["ssm_c_re"])
    o["c_im"] = c_layout(inp["ssm_c_im"])
    o["ssm_d"] = np.ascontiguousarray(inp["ssm_d"].reshape(L, 4, 128).transpose(0, 2, 1))
    o["w_glu"] = inp["w_glu"]
    o["b_glu"] = np.ascontiguousarray(inp["b_glu"].reshape(L, 8, 128).transpose(0, 2, 1))
    o["w_branch"] = inp["w_branch"].reshape(L, 1536, 1024)
    o["w_out"] = inp["w_out"]
    for n in ("ln1_g", "ln1_b", "ln2_g", "ln2_b"):
        o[n] = inp[n]
    o["ffn_w1"] = inp["ffn_w1"]
    o["ffn_w3"] = inp["ffn_w3"]
    o["ffn_w2"] = inp["ffn_w2"]
    o["moe_router"] = inp["moe_router"]
    o["moe_router_b"] = inp["moe_router_b"]
    o["moe_w1"] = inp["moe_w1"].reshape(16, 1024, DFF)
    o["moe_w3"] = inp["moe_w3"].reshape(16, 1024, DFF)
    o["moe_w2"] = inp["moe_w2"].reshape(16, DFF, 1024)
    return o


class K:
    pass


_U = [0]


def SBT(nc, name, shape, dt):
    _U[0] += 1
    return nc.sbuf_tensor(f"{name}_u{_U[0]}", list(shape), dt)


def build_program(n_layers=4, stop_after=None, dbg=()):
    nc = bass.Bass("TRN2", target_bir_lowering=False)
    k = K()
    k.nc = nc
    k.dbg = dbg

    def din(name, shape, dt=F32):
        return nc.dram_tensor(name, list(shape), dt, kind="ExternalInput").ap()

    def dscr(name, shape, dt):
        kind = "ExternalOutput" if name in dbg else "Internal"
        return nc.dram_tensor(name, list(shape), dt, kind=kind).ap()

    L = 4
    k.x = din("x", [T, D])
    k.y = nc.dram_tensor("y", [T, D], F32, kind="ExternalOutput").ap()
    k.w_in = din("w_in", [L, D, INW])
    k.b_in_fm = din("b_in_fm", [L, 128, 44])
    k.b_in = din("b_in", [L, INW])
    k.w_pool = din("w_pool", [L, 128, 512])
    k.pool_scale = din("pool_scale", [L, 128, 4])
    for n in ("a_re", "a_im", "log_dt"):
        setattr(k, n, din(n, [L, 128, 16]))
    for n in ("b_re", "b_im", "c_re", "c_im"):
        setattr(k, n, din(n, [L, 128, 2048]))
    k.ssm_d = din("ssm_d", [L, 128, 4])
    k.w_glu = din("w_glu", [L, 512, 1024])
    k.b_glu = din("b_glu", [L, 128, 8])
    k.w_branch = din("w_branch", [L, 1536, 1024])
    k.w_out = din("w_out", [L, D, D])
    for n in ("ln1_g", "ln1_b", "ln2_g", "ln2_b"):
        setattr(k, n, din(n, [L, D]))
    k.ffn_w1 = din("ffn_w1", [2, D, DFF])
    k.ffn_w3 = din("ffn_w3", [2, D, DFF])
    k.ffn_w2 = din("ffn_w2", [2, DFF, D])
    k.moe_router = din("moe_router", [2, D, 8])
    k.moe_router_b = din("moe_router_b", [2, 8])
    k.moe_w1 = din("moe_w1", [16, D, DFF])
    k.moe_w3 = din("moe_w3", [16, D, DFF])
    k.moe_w2 = din("moe_w2", [16, DFF, D])
    k.c_pw = din("c_pw", [128, 1536])
    k.c_ind = din("c_ind", [128, 8192])
    k.c_expb = din("c_expb", [128, 136])
    k.c_cbtri = din("c_cbtri", [128, 128])
    k.c_negq = din("c_negq", [128, 512])
    k.c_ident = din("c_ident", [128, 128])
    k.c_iota = din("c_iota", [1, T])
    k.c_sel8 = din("c_sel8", [8, 1024])
    k.s_h = dscr("s_h", [T, D], F32)
    k.s_ua = dscr("s_ua", [T, 512], BF16)
    k.s_v = dscr("s_v", [T, 512], BF16)
    k.s_q = dscr("s_q", [512, T], BF16)
    k.s_k = dscr("s_k", [512, T], BF16)
    k.s_uc = dscr("s_uc", [512, T], BF16)
    k.s_g = dscr("s_g", [3072, T], BF16)
    k.s_ya = dscr("s_ya", [512, T], BF16)
    k.s_yb = dscr("s_yb", [512, T], BF16)
    k.s_yc = dscr("s_yc", [512, T], BF16)
    k.s_gate = dscr("s_gate", [T, 8], F32)

    with ExitStack() as es:
        P = Prog(nc, es)
        k.P = P
        sbt = lambda n, s, d: es.enter_context(SBT(nc, n, list(s), d))
        k.ps = [es.enter_context(nc.psum_tensor(f"ps{i}", [128, 512], F32)) for i in range(8)]
        k.hT = sbt("hT", [128, 8, T], BF16)
        k.ident = sbt("ident", [128, 128], F32)
        k.identb = sbt("identb", [128, 128], BF16)
        k.cone = sbt("cone", [128, 1], F32)
        k.cnmag = sbt("cnmag", [128, 1], F32)
        k.cpmag = sbt("cpmag", [128, 1], F32)
        P.op("dve", lambda e: e.memset(k.cpmag[:], MAGIC), writes=["cpmag"])
        P.op("dve", lambda e: e.memset(k.cone[:], 1.0), writes=["cone"])
        P.op("dve", lambda e: e.memset(k.cnmag[:], -MAGIC), writes=["cnmag"])
        P.dma("sp", k.ident[:], k.c_ident, writes=["ident"], semkey="c0")
        P.dma("pool", k.identb[:], k.c_ident, writes=["identb"], semkey="c1")

        stage_init(k)
        for l in range(n_layers):
            stages = [("s1", stage_inproj), ("s2", stage_pool), ("s3", stage_attn), ("s4", stage_s5),
                      ("s6", stage_merge), ("s8", stage_ffn)]
            done = False
            for nm, fn in stages:
                P.barrier()
                fn(k, l)
                if stop_after == (l, nm):
                    done = True
                    break
            if done:
                break
        P.barrier()
        for e in ("sp", "act", "pool", "dve", "pe"):
            P.wait_all(e)
        print("instr counts", P.nins, "sems", P.nsem)
    return nc


def to_hT(k, src, src_key, tt):
    P = k.P
    for half in range(2):
        ps = k.ps[6 + half]
        pk = ("ps", 6 + half)
        for j in range(4):
            dc = half * 4 + j
            P.op("pe", lambda e, ps=ps, j=j, dc=dc: e.transpose(ps[:, j * 128:(j + 1) * 128], src[:, dc * 128:(dc + 1) * 128], k.ident[:]),
                 reads=[src_key, "ident"], writes=[pk], pe_chain=(j > 0))
        P.op("act", lambda e, ps=ps, half=half: e.activation(
            k.hT[:, half * 4:(half + 1) * 4, tt * 128:(tt + 1) * 128],
            ps[:].rearrange("p (j t) -> p j t", j=4), AF.Copy),
            reads=[pk], writes=[("hT", tt)])


def stage_init(k):
    P, nc = k.P, k.nc
    with ExitStack() as es:
        xt = [es.enter_context(SBT(nc, f"xt{i}", [128, D], F32)) for i in range(2)]
        for tt in range(NT):
            b = tt % 2
            P.dma("sp", xt[b][:], k.x[tt * 128:(tt + 1) * 128, :], writes=[("xt", b)], semkey=("xt", b))
            to_hT(k, xt[b], ("xt", b), tt)
        P.barrier()


def hT_keys():
    return [("hT", t) for t in range(NT)]


def stage_inproj(k, l):
    P, nc = k.P, k.nc
    with ExitStack() as es:
        sbt = lambda n, s, d: es.enter_context(SBT(nc, n, list(s), d))
        wb = [sbt(f"wblk{i}", [128, 8, 512], BF16) for i in range(3)]
        ot = [sbt(f"ot{i}", [128, 512], BF16) for i in range(4)]
        bfm = sbt("bfm", [128, 44], F32)
        bbc = sbt("bbc", [128, 2, 512], F32)
        P.dma("sp", bfm[:], k.b_in_fm[l], writes=["bfm"], semkey="bfm")
        P.dma("sp", bbc[:, 0, :], k.b_in[l:l + 1, 0:512].partition_broadcast(128), writes=["bbc"], semkey="bbc")
        P.dma("sp", bbc[:, 1, :], k.b_in[l:l + 1, 1536:2048].partition_broadcast(128), writes=["bbc"], semkey="bbc")
        wsrc = k.w_in[l].rearrange("(k p) c -> p k c", p=128)
        oi = 0
        pi = 0
        for blk in range(11):
            wbi = blk % 3
            P.dma("pool", wb[wbi][:], wsrc[:, :, blk * 512:(blk + 1) * 512], writes=[("wblk", wbi)], semkey=("wblk", wbi))
            if blk in (0, 3):
                dst = k.s_ua if blk == 0 else k.s_v
                bi = 0 if blk == 0 else 1
                for tt in range(NT):
                    ps = k.ps[pi % 6]
                    pk = ("ps", pi % 6)
                    pi += 1
                    for kk in range(8):
                        P.op("pe", lambda e, ps=ps, kk=kk, tt=tt, wbi=wbi: e.matmul(
                            ps[:], k.hT[:, kk, tt * 128:(tt + 1) * 128], wb[wbi][:, kk, :], start=(kk == 0), stop=(kk == 7)),
                            reads=[("hT", tt), ("wblk", wbi)], writes=[pk], pe_chain=(kk > 0))
                    o = oi % 4
                    oi += 1
                    P.op("dve", lambda e, ps=ps, o=o, bi=bi: e.tensor_tensor(ot[o][:], ps[:], bbc[:, bi, :], ALU.add),
                         reads=[pk, "bbc"], writes=[("ot", o)])
                    P.dma("sp", dst[tt * 128:(tt + 1) * 128, :], ot[o][:], reads=[("ot", o)], semkey=("ot", o))
            else:
                for c in range(4):
                    col = blk * 4 + c
                    if blk == 1:
                        dst, func = k.s_q[c * 128:(c + 1) * 128, :], AF.Identity
                    elif blk == 2:
                        dst, func = k.s_k[c * 128:(c + 1) * 128, :], AF.Identity
                    elif blk == 4:
                        dst, func = k.s_uc[c * 128:(c + 1) * 128, :], AF.Identity
                    else:
                        r0 = (blk - 5) * 512 + c * 128
                        dst, func = k.s_g[r0:r0 + 128, :], AF.Sigmoid
                    for tq in range(4):
                        ps = k.ps[pi % 6]
                        pk = ("ps", pi % 6)
                        pi += 1
                        for kk in range(8):
                            P.op("pe", lambda e, ps=ps, kk=kk, tq=tq, wbi=wbi, c=c: e.matmul(
                                ps[:], wb[wbi][:, kk, c * 128:(c + 1) * 128], k.hT[:, kk, tq * 512:(tq + 1) * 512],
                                start=(kk == 0), stop=(kk == 7)),
                                reads=[("hT", 4 * tq + j) for j in range(4)] + [("wblk", wbi)], writes=[pk], pe_chain=(kk > 0))
                        o = oi % 4
                        oi += 1
                        P.op("act", lambda e, ps=ps, o=o, col=col, func=func: e.activation(
                            ot[o][:], ps[:], func, bias=bfm[:, col:col + 1], scale=1.0),
                            reads=[pk, "bfm"], writes=[("ot", o)])
                        P.dma("sp", dst[:, tq * 512:(tq + 1) * 512], ot[o][:], reads=[("ot", o)], semkey=("ot", o))
        P.barrier()


def stage_pool(k, l):
    P, nc = k.P, k.nc
    with ExitStack() as es:
        sbt = lambda n, s, d: es.enter_context(SBT(nc, n, list(s), d))
        ua = sbt("ua", [128, NT, 512], BF16)
        pw = sbt("pw", [128, 4, 3, 128], BF16)
        wp = sbt("wp", [128, 4, 128], BF16)
        psc = sbt("psc", [128, 4], F32)
        zin = [sbt(f"zin{i}", [128, 512], BF16) for i in range(2)]
        yo = [sbt(f"yo{i}", [128, 512], BF16) for i in range(2)]
        P.dma("sp", ua[:], k.s_ua.rearrange("(t p) c -> p t c", p=128), writes=["ua"], semkey="ua")
        P.dma("pool", pw[:], k.c_pw.rearrange("p (g k t) -> p g k t", g=4, k=3), writes=["pw"], semkey="pw")
        P.dma("pool", wp[:], k.w_pool[l].rearrange("p (g d) -> p g d", g=4), writes=["wp"], semkey="wp")
        P.dma("sp", psc[:], k.pool_scale[l], writes=["psc"], semkey="psc")
        it = 0
        for g in range(4):
            for tq in range(4):
                b = it % 2
                it += 1
                psA, psB = k.ps[b], k.ps[2 + b]
                for j in range(4):
                    tt = tq * 4 + j
                    kind = 0 if tt == 0 else 1
                    P.op("pe", lambda e, psA=psA, j=j, tt=tt, g=g, kind=kind: e.matmul(
                        psA[:, j * 128:(j + 1) * 128], ua[:, tt, g * 128:(g + 1) * 128], pw[:, g, kind, :],
                        start=True, stop=(tt == 0)), reads=["ua", "pw"], writes=[("ps", b)], pe_chain=(j > 0))
                    if tt > 0:
                        P.op("pe", lambda e, psA=psA, j=j, tt=tt, g=g: e.matmul(
                            psA[:, j * 128:(j + 1) * 128], ua[:, tt - 1, g * 128:(g + 1) * 128], pw[:, g, 2, :],
                            start=False, stop=True), reads=["ua", "pw"], writes=[("ps", b)], pe_chain=True)
                P.op("act", lambda e, psA=psA, b=b: e.activation(zin[b][:], psA[:], AF.Copy),
                     reads=[("ps", b)], writes=[("zin", b)])
                P.op("pe", lambda e, psB=psB, b=b, g=g: e.matmul(psB[:], wp[:, g, :], zin[b][:], start=True, stop=True),
                     reads=["wp", ("zin", b)], writes=[("ps", 2 + b)])
                P.op("act", lambda e, psB=psB, b=b, g=g: e.activation(yo[b][:], psB[:], AF.Copy, scale=psc[:, g:g + 1]),
                     reads=[("ps", 2 + b), "psc"], writes=[("yo", b)])
                P.dma("sp", k.s_ya[g * 128:(g + 1) * 128, tq * 512:(tq + 1) * 512], yo[b][:], reads=[("yo", b)], semkey=("yo", b))
        P.barrier()


def stage_attn(k, l):
    P, nc = k.P, k.nc
    with ExitStack() as es:
        sbt = lambda n, s, d: es.enter_context(SBT(nc, n, list(s), d))
        qT = sbt("qT", [128, 4, T], BF16)
        kT = sbt("kT", [128, 4, T], BF16)
        va = sbt("va", [128, NT, 8, 65], BF16)
        ind = sbt("ind", [128, 64, 128], BF16)
        expb = sbt("expb", [128, 8, 17], F32)
        cbt = sbt("cbt", [128, 128], BF16)
        negq = sbt("negq", [128, 8, 64], F32)
        ksum = sbt("ksum", [128, 4, 8], F32)
        kmT = sbt("kmT", [128, 4, 8], BF16)
        mbT = sbt("mbT", [128, T], BF16)
        gm = [sbt(f"gm{i}", [128, 64], F32) for i in range(2)]
        m8 = [sbt(f"m8{i}", [128, 8], F32) for i in range(2)]
        selb = [sbt(f"selb{i}", [128, 64], F32) for i in range(2)]
        pT = [sbt(f"pT{i}", [128, 256], BF16) for i in range(3)]
        rec = [sbt(f"rec{i}", [128, 1], F32) for i in range(4)]
        ybt = [sbt(f"ybt{i}", [128, 2, 512], BF16) for i in range(2)]
        ybo = [sbt(f"ybo{i}", [128, 4, 256], BF16) for i in range(2)]
        P.dma("sp", qT[:], k.s_q.rearrange("(c p) t -> p c t", p=128), writes=["qT"], semkey="qT")
        P.dma("sp", kT[:], k.s_k.rearrange("(c p) t -> p c t", p=128), writes=["kT"], semkey="kT")
        P.op("dve", lambda e: e.memset(va[:], 1.0), writes=["va"])
        P.op("dve", lambda e: e.memset(mbT[:], 0.0), writes=["mbT"])
        for tt in range(NT):
            P.dma("sp", va[:, tt, :, 0:64], k.s_v[tt * 128:(tt + 1) * 128, :].rearrange("p (h d) -> p h d", h=8),
                  writes=["va"], semkey="va")
        P.dma("pool", ind[:], k.c_ind.rearrange("p (j t) -> p j t", j=64), writes=["ind"], semkey="ind")
        P.dma("sp", expb[:], k.c_expb.rearrange("p (h m) -> p h m", h=8), writes=["expb"], semkey="expb")
        P.dma("pool", cbt[:], k.c_cbtri, writes=["cbt"], semkey="cbt")
        P.dma("sp", negq[:], k.c_negq.rearrange("p (q c) -> p q c", q=8), writes=["negq"], semkey="negq")
        ph = 9
        P.op("dve", lambda e: e.tensor_reduce(ksum[:], kT[:].rearrange("p c (n s) -> p c n s", s=256), AX.X, ALU.add),
             reads=["kT"], writes=["ksum"])
        P.op("act", lambda e: e.activation(kmT[:], ksum[:], AF.Copy, scale=1.0 / 256), reads=["ksum"], writes=["kmT"])
        psG = k.ps[7]
        for tt in range(8, NT):
            qb = tt // 2
            b = tt % 2
            for h in range(8):
                c, base = h // 2, (h % 2) * 64
                P.op("pe", lambda e, h=h, c=c, base=base, tt=tt: e.matmul(
                    psG[:, h * 8:(h + 1) * 8], qT[base:base + 64, c, tt * 128:(tt + 1) * 128], kmT[base:base + 64, c, :],
                    start=True, stop=True), reads=["qT", "kmT"], writes=[("ps", 7)])
            P.op("dve", lambda e, b=b, qb=qb: e.tensor_tensor(gm[b][:], psG[:, 0:64], negq[:, qb, :], ALU.add),
                 reads=[("ps", 7), "negq"], writes=[("gm", b)])
            for h in range(8):
                P.op("dve", lambda e, b=b, h=h: e.max(m8[b][:], gm[b][:, h * 8:(h + 1) * 8]),
                     reads=[("gm", b)], writes=[("m8", b)])
                P.op("dve", lambda e, b=b, h=h: e.tensor_scalar(selb[b][:, h * 8:(h + 1) * 8], gm[b][:, h * 8:(h + 1) * 8],
                                                               m8[b][:, 2:3], NEGM, ALU.is_lt, ALU.mult),
                     reads=[("gm", b), ("m8", b)], writes=[("selb", b)])
            P.op("pe", lambda e, b=b: e.transpose(psG[0:64, 128:256], selb[b][:], k.ident[:]),
                 reads=[("selb", b), "ident"], writes=[("ps", 7)])
            P.op("act", lambda e, tt=tt: e.activation(mbT[0:64, tt * 128:(tt + 1) * 128], psG[0:64, 128:256], AF.Copy),
                 reads=[("ps", 7)], writes=["mbT"])
        if ph == 1:
            P.barrier()
            return
        psTb = k.ps[3][:].bitcast(BF16)

        def make_unit(qb, h, kt, sb_, pb, ris):
            Q0 = qb * 256
            c, base = h // 2, (h % 2) * 64
            ob = h % 2
            psO = [k.ps[4 + 2 * ob], k.ps[5 + 2 * ob]]
            pok = [("ps", 4 + 2 * ob), ("ps", 5 + 2 * ob)]
            psS = k.ps[sb_]
            psk = ("ps", sb_)
            own_a = kt == 2 * qb
            own_b = kt == 2 * qb + 1
            kcols = slice(kt * 128, (kt + 1) * 128)
            js = [1] if own_b else [0, 1]
            yb = ybt[qb % 2]
            ybk = ("ybt", qb % 2)

            def score():
                if own_b:
                    P.op("pe", lambda e: e.matmul(
                        psS[:, 128:256], kT[base:base + 64, c, kcols], qT[base:base + 64, c, Q0 + 128:Q0 + 256],
                        start=True, stop=False), reads=["kT", "qT"], writes=[psk])
                    P.op("pe", lambda e: e.matmul(psS[:, 128:256], k.identb[:], cbt[:], start=False, stop=True),
                         reads=["identb", "cbt"], writes=[psk], pe_chain=True)
                elif own_a:
                    P.op("pe", lambda e: e.matmul(
                        psS[:, 0:128], kT[base:base + 64, c, kcols], qT[base:base + 64, c, Q0:Q0 + 128],
                        start=True, stop=False), reads=["kT", "qT"], writes=[psk])
                    P.op("pe", lambda e: e.matmul(psS[:, 0:128], k.identb[:], cbt[:], start=False, stop=True),
                         reads=["identb", "cbt"], writes=[psk], pe_chain=True)
                    P.op("pe", lambda e: e.matmul(
                        psS[:, 128:256], kT[base:base + 64, c, kcols], qT[base:base + 64, c, Q0 + 128:Q0 + 256],
                        start=True, stop=True), reads=["kT", "qT"], writes=[psk], pe_chain=True)
                else:
                    masked = qb >= 4
                    P.op("pe", lambda e: e.matmul(
                        psS[:, 0:256], kT[base:base + 64, c, kcols], qT[base:base + 64, c, Q0:Q0 + 256],
                        start=True, stop=(not masked)), reads=["kT", "qT"], writes=[psk])
                    if masked:
                        n = kt // 2
                        P.op("pe", lambda e: e.matmul(
                            psS[:, 0:256], ind[:, h * 8 + n, :], mbT[:, Q0:Q0 + 256], start=False, stop=True),
                            reads=["ind", "mbT"], writes=[psk], pe_chain=True)

            def exps():
                m = 2 * qb - kt + 1
                c0, c1 = js[0] * 128, (js[-1] + 1) * 128
                P.op("act", lambda e: e.activation(
                    pT[pb][:, c0:c1], psS[:, c0:c1], AF.Exp, bias=expb[:, h, m:m + 1], scale=0.125),
                    reads=[psk, "expb"], writes=[("pT", pb, j) for j in js])

            def pv():
                for j in js:
                    last = (kt == 2 * qb) if j == 0 else (kt == 2 * qb + 1)
                    P.op("pe", lambda e, j=j, last=last: e.matmul(
                        psO[j][:, 0:65], pT[pb][:, j * 128:(j + 1) * 128], va[:, kt, h, :], start=(kt == 0), stop=last),
                        reads=[("pT", pb, j), "va"], writes=[pok[j]], pe_chain=(kt > 0))
                if not own_b:
                    return
                for j in range(2):
                    r = ris[j]
                    P.op("dve", lambda e, r=r, j=j: e.reciprocal(rec[r][:], psO[j][:, 64:65]),
                         reads=[pok[j]], writes=[("rec", r)])
                    P.op("dve", lambda e, r=r, j=j: e.tensor_scalar(
                        yb[:, j, h * 64:(h + 1) * 64], psO[j][:, 0:64], rec[r][:, 0:1], None, ALU.mult),
                        reads=[pok[j], ("rec", r)], writes=[ybk])
                if h != 7:
                    return
                yo = ybo[qb % 2]
                yok = ("ybo", qb % 2)
                for j in range(2):
                    for cc in range(4):
                        P.op("pe", lambda e, j=j, cc=cc: e.transpose(
                            psTb[:, (j * 4 + cc) * 128:(j * 4 + cc + 1) * 128], yb[:, j, cc * 128:(cc + 1) * 128], k.identb[:]),
                            reads=[ybk, "identb"], writes=[("ps", 3)], pe_chain=(j + cc > 0))
                for j in range(2):
                    P.op("act", lambda e, j=j: e.activation(
                        yo[:, :, j * 128:(j + 1) * 128], psTb[:, j * 512:(j + 1) * 512].rearrange("p (c t) -> p c t", c=4), AF.Copy),
                        reads=[("ps", 3)], writes=[yok])
                P.dma("sp", k.s_yb.rearrange("(c p) t -> p c t", p=128)[:, :, Q0:Q0 + 256], yo[:], reads=[yok], semkey=yok)
            return score, exps, pv

        units = []
        si = 0
        ri = 0
        for qb in range(8):
            for h in range(8):
                for kt in range(2 * qb + 2):
                    ris = (ri % 4, (ri + 1) % 4)
                    if kt == 2 * qb + 1:
                        ri += 2
                    units.append(make_unit(qb, h, kt, si % 3, si % 3, ris))
                    si += 1
        LAG = 1
        for i, u in enumerate(units):
            u[0]()
            u[1]()
            if i >= LAG:
                units[i - LAG][2]()
        for i in range(max(0, len(units) - LAG), len(units)):
            units[i][2]()
        P.barrier()


def stage_s5(k, l):
    P, nc = k.P, k.nc
    INV2PI = 0.15915494309189535
    with ExitStack() as es:
        sbt = lambda n, s, d: es.enter_context(SBT(nc, n, list(s), d))
        ucT = sbt("ucT", [128, 4, T], BF16)
        blr = sbt("blr", [128, 16, 128], BF16)
        bli = sbt("bli", [128, 16, 128], BF16)
        cpr = sbt("cpr", [128, 16, 128], F32)
        cpi = sbt("cpi", [128, 16, 128], F32)
        iota = sbt("iota", [128, T], F32)
        bglu = sbt("bglu", [128, 8], F32)
        dsk = sbt("dsk", [128, 4], F32)
        gel = sbt("gel", [128, 4, T], BF16)
        sm = {}
        for n in ("ar", "ai", "ldt", "dt", "ad", "mag", "th", "u", "kk", "thf", "sn", "sh", "cs", "lr", "li",
                  "den", "t1", "t2", "rden", "nr", "fr", "fi", "nfr"):
            sm[n] = sbt("sm_" + n, [128, 16], F32)
        P.dma("sp", ucT[:], k.s_uc.rearrange("(c p) t -> p c t", p=128), writes=["ucT"], semkey="ucT")
        P.dma("pool", blr[:], k.b_re[l].rearrange("p (s n) -> p s n", s=16), writes=["blr"], semkey="blr")
        P.dma("pool", bli[:], k.b_im[l].rearrange("p (s n) -> p s n", s=16), writes=["bli"], semkey="bli")
        P.dma("sp", iota[:], k.c_iota.partition_broadcast(128), writes=["iota"], semkey="iota")
        P.dma("sp", bglu[:], k.b_glu[l], writes=["bglu"], semkey="bglu")
        P.dma("sp", dsk[:], k.ssm_d[l], writes=["dsk"], semkey="dsk")
        P.dma("sp", sm["ar"][:], k.a_re[l], writes=["ar"], semkey="ar")
        P.dma("sp", sm["ai"][:], k.a_im[l], writes=["ai"], semkey="ai")
        P.dma("sp", sm["ldt"][:], k.log_dt[l], writes=["ldt"], semkey="ldt")

        def S(eng, fn, r, w):
            P.op(eng, fn, reads=r, writes=w)
        a = lambda n: sm[n][:]
        S("act", lambda e: e.activation(a("dt"), a("ldt"), AF.Exp), ["ldt"], ["dt"])
        S("dve", lambda e: e.tensor_tensor(a("ad"), a("ar"), a("dt"), ALU.mult), ["ar", "dt"], ["ad"])
        S("act", lambda e: e.activation(a("mag"), a("ad"), AF.Exp), ["ad"], ["mag"])
        S("dve", lambda e: e.tensor_tensor(a("th"), a("ai"), a("dt"), ALU.mult), ["ai", "dt"], ["th"])
        S("dve", lambda e: e.tensor_scalar(a("u"), a("th"), INV2PI, MAGIC, ALU.mult, ALU.add), ["th"], ["u"])
        S("dve", lambda e: e.tensor_scalar(a("kk"), a("u"), MAGIC, None, ALU.subtract), ["u"], ["kk"])
        S("dve", lambda e: e.scalar_tensor_tensor(a("thf"), a("th"), INV2PI, a("kk"), ALU.mult, ALU.subtract), ["th", "kk"], ["thf"])
        S("act", lambda e: e.activation(a("sn"), a("thf"), AF.Sin, scale=TWO_PI_LO), ["thf"], ["sn"])
        S("act", lambda e: e.activation(a("sh"), a("thf"), AF.Sin, scale=PI_LO), ["thf"], ["sh"])
        S("dve", lambda e: e.tensor_tensor(a("cs"), a("sh"), a("sh"), ALU.mult), ["sh"], ["cs"])
        S("dve", lambda e: e.tensor_scalar(a("cs"), a("cs"), -2.0, 1.0, ALU.mult, ALU.add), ["cs"], ["cs"])
        S("dve", lambda e: e.tensor_tensor(a("lr"), a("mag"), a("cs"), ALU.mult), ["mag", "cs"], ["lr"])
        S("dve", lambda e: e.tensor_tensor(a("li"), a("mag"), a("sn"), ALU.mult), ["mag", "sn"], ["li"])
        S("dve", lambda e: e.tensor_tensor(a("den"), a("ar"), a("ar"), ALU.mult), ["ar"], ["den"])
        S("dve", lambda e: e.tensor_tensor(a("t1"), a("ai"), a("ai"), ALU.mult), ["ai"], ["t1"])
        S("dve", lambda e: e.tensor_tensor(a("den"), a("den"), a("t1"), ALU.add), ["den", "t1"], ["den"])
        S("dve", lambda e: e.reciprocal(a("rden"), a("den")), ["den"], ["rden"])
        S("dve", lambda e: e.tensor_scalar(a("nr"), a("lr"), -1.0, None, ALU.add), ["lr"], ["nr"])
        S("dve", lambda e: e.tensor_tensor(a("t1"), a("nr"), a("ar"), ALU.mult), ["nr", "ar"], ["t1"])
        S("dve", lambda e: e.tensor_tensor(a("t2"), a("li"), a("ai"), ALU.mult), ["li", "ai"], ["t2"])
        S("dve", lambda e: e.tensor_tensor(a("t1"), a("t1"), a("t2"), ALU.add), ["t1", "t2"], ["t1"])
        S("dve", lambda e: e.tensor_tensor(a("fr"), a("t1"), a("rden"), ALU.mult), ["t1", "rden"], ["fr"])
        S("dve", lambda e: e.tensor_tensor(a("t1"), a("li"), a("ar"), ALU.mult), ["li", "ar"], ["t1"])
        S("dve", lambda e: e.tensor_tensor(a("t2"), a("nr"), a("ai"), ALU.mult), ["nr", "ai"], ["t2"])
        S("dve", lambda e: e.tensor_tensor(a("t1"), a("t1"), a("t2"), ALU.subtract), ["t1", "t2"], ["t1"])
        S("dve", lambda e: e.tensor_tensor(a("fi"), a("t1"), a("rden"), ALU.mult), ["t1", "rden"], ["fi"])
        S("dve", lambda e: e.tensor_scalar(a("nfr"), a("fr"), -1.0, None, ALU.mult), ["fr"], ["nfr"])
        with ExitStack() as es2:
            clr = es2.enter_context(SBT(nc, "clr", [128, 16, 128], F32))
            cli = es2.enter_context(SBT(nc, "cli", [128, 16, 128], F32))
            ctm = es2.enter_context(SBT(nc, "ctm", [128, 16, 128], F32))
            P.dma("sp", clr[:], k.c_re[l].rearrange("p (s n) -> p s n", s=16), writes=["clr"], semkey="clr")
            P.dma("sp", cli[:], k.c_im[l].rearrange("p (s n) -> p s n", s=16), writes=["cli"], semkey="cli")
            for st in range(16):
                fr_, fi_, nfr_ = sm["fr"][:, st:st + 1], sm["fi"][:, st:st + 1], sm["nfr"][:, st:st + 1]
                S("dve", lambda e, st=st, fi_=fi_: e.tensor_scalar(ctm[:, st, :], cli[:, st, :], fi_, None, ALU.mult),
                  ["cli", "fi"], [("ctm", st)])
                S("dve", lambda e, st=st, fr_=fr_: e.scalar_tensor_tensor(cpr[:, st, :], clr[:, st, :], fr_, ctm[:, st, :], ALU.mult, ALU.subtract),
                  ["clr", "fr", ("ctm", st)], [("cpr", st)])
                S("dve", lambda e, st=st, fi_=fi_: e.tensor_scalar(ctm[:, st, :], clr[:, st, :], fi_, None, ALU.mult),
                  ["clr", "fi"], [("ctm", st)])
                S("dve", lambda e, st=st, nfr_=nfr_: e.scalar_tensor_tensor(cpi[:, st, :], cli[:, st, :], nfr_, ctm[:, st, :], ALU.mult, ALU.subtract),
                  ["cli", "nfr", ("ctm", st)], [("cpi", st)])
            P.barrier()
        with ExitStack() as es3:
            sbt3 = lambda n, s, d: es3.enter_context(SBT(nc, n, list(s), d))
            big = {}
            for n in ("A", "B", "FR", "XR", "XI", "GR", "GI"):
                big[n] = sbt3("big_" + n, [128, T], F32)
            SN2 = [sbt3(f"big_SN{i}", [128, T], F32) for i in range(2)]
            CS2 = [sbt3(f"big_CS{i}", [128, T], F32) for i in range(2)]
            tc = [sbt3(f"tc{i}", [128, 512], F32) for i in range(8)]
            g = lambda n: big[n][:]
            tcs = {"i": 0}
            xrk = [("XR", i) for i in range(4)]
            xik = [("XI", i) for i in range(4)]

            def tables_a(st):
                thf_ = sm["thf"][:, st:st + 1]
                S("act", lambda e: e.activation(g("A"), iota[:], AF.Identity, bias=k.cpmag[:], scale=thf_), ["iota", "thf", "cpmag"], ["A"])
                S("act", lambda e: e.activation(g("B"), g("A"), AF.Identity, bias=k.cnmag[:], scale=1.0), ["A", "cnmag"], ["B"])

            def tables_b(st):
                p = st % 2
                thf_ = sm["thf"][:, st:st + 1]
                sn, cs = SN2[p], CS2[p]
                S("dve", lambda e: e.scalar_tensor_tensor(g("FR"), iota[:], thf_, g("B"), ALU.mult, ALU.subtract), ["iota", "thf", "B"], ["FR"])
                S("act", lambda e: e.activation(sn[:], g("FR"), AF.Sin, scale=TWO_PI_LO), ["FR"], [("SN", p)])
                S("act", lambda e: e.activation(cs[:], g("FR"), AF.Sin, scale=PI_LO), ["FR"], [("CS", p)])
                S("act", lambda e: e.activation(cs[:], cs[:], AF.Square), [("CS", p)], [("CS", p)])
                S("act", lambda e: e.activation(cs[:], cs[:], AF.Identity, bias=k.cone[:], scale=-2.0), [("CS", p), "cone"], [("CS", p)])

            def forward(st):
                cc = st // 4
                p = st % 2
                sn, cs = SN2[p], CS2[p]
                for tq in range(4):
                    cs_ = slice(tq * 512, (tq + 1) * 512)
                    pb = (tq % 2) * 2
                    psXr, psXi = k.ps[pb], k.ps[pb + 1]
                    S("pe", lambda e, psXr=psXr, cs_=cs_: e.matmul(psXr[:], blr[:, st, :], ucT[:, cc, cs_], start=True, stop=True),
                      ["blr", "ucT"], [("ps", pb)])
                    S("pe", lambda e, psXi=psXi, cs_=cs_: e.matmul(psXi[:], bli[:, st, :], ucT[:, cc, cs_], start=True, stop=True),
                      ["bli", "ucT"], [("ps", pb + 1)])
                    t = [tc[(tcs["i"] + i) % 8] for i in range(4)]
                    tk = [("tc", (tcs["i"] + i) % 8) for i in range(4)]
                    tcs["i"] += 4
                    S("dve", lambda e, t=t, psXr=psXr, cs_=cs_: e.tensor_tensor(t[0][:], psXr[:], cs[:, cs_], ALU.mult), [("ps", pb), ("CS", p)], [tk[0]])
                    S("dve", lambda e, t=t, psXi=psXi, cs_=cs_: e.tensor_tensor(t[1][:], psXi[:], sn[:, cs_], ALU.mult), [("ps", pb + 1), ("SN", p)], [tk[1]])
                    S("pool", lambda e, t=t, cs_=cs_: e.tensor_tensor(big["XR"][:, cs_], t[0][:], t[1][:], ALU.add), [tk[0], tk[1]], [("XR", tq)])
                    S("dve", lambda e, t=t, psXi=psXi, cs_=cs_: e.tensor_tensor(t[2][:], psXi[:], cs[:, cs_], ALU.mult), [("ps", pb + 1), ("CS", p)], [tk[2]])
                    S("dve", lambda e, t=t, psXr=psXr, cs_=cs_: e.tensor_tensor(t[3][:], psXr[:], sn[:, cs_], ALU.mult), [("ps", pb), ("SN", p)], [tk[3]])
                    S("pool", lambda e, t=t, cs_=cs_: e.tensor_tensor(big["XI"][:, cs_], t[2][:], t[3][:], ALU.subtract), [tk[2], tk[3]], [("XI", tq)])

            def scan_back(st):
                cc = st // 4
                p = st % 2
                sn, cs = SN2[p], CS2[p]
                snk, csk = ("SN", p), ("CS", p)
                rb = sm["mag"][:, st:st + 1].to_broadcast([128, T])
                S("dve", lambda e: e.tensor_tensor_scan(g("GR"), rb, g("XR"), 0.0, ALU.mult, ALU.add), ["mag"] + xrk, ["GR"])
                S("dve", lambda e: e.tensor_tensor_scan(g("GI"), rb, g("XI"), 0.0, ALU.mult, ALU.add), ["mag"] + xik, ["GI"])
                S("dve", lambda e: e.tensor_tensor(g("A"), g("GR"), cs[:], ALU.mult), ["GR", csk], ["A"])
                S("dve", lambda e: e.tensor_tensor(g("B"), g("GI"), sn[:], ALU.mult), ["GI", snk], ["B"])
                S("dve", lambda e: e.tensor_tensor(g("XR"), g("A"), g("B"), ALU.subtract), ["A", "B"], xrk)
                S("dve", lambda e: e.tensor_tensor(g("FR"), g("GR"), sn[:], ALU.mult), ["GR", snk], ["FR"])
                S("dve", lambda e: e.tensor_tensor(g("A"), g("GI"), cs[:], ALU.mult), ["GI", csk], ["A"])
                S("dve", lambda e: e.tensor_tensor(g("XI"), g("FR"), g("A"), ALU.add), ["FR", "A"], xik)
                for tq in range(4):
                    cs_ = slice(tq * 512, (tq + 1) * 512)
                    psY = k.ps[4 + tq]
                    S("pe", lambda e, psY=psY, cs_=cs_: e.matmul(psY[:], cpr[:, st, :], big["XR"][:, cs_], start=(st % 4 == 0), stop=False),
                      [("cpr", st)] + xrk, [("ps", 4 + tq)])
                    S("pe", lambda e, psY=psY, cs_=cs_: e.matmul(psY[:], cpi[:, st, :], big["XI"][:, cs_], start=False, stop=(st % 4 == 3)),
                      [("cpi", st)] + xik, [("ps", 4 + tq)])
                if st % 4 == 3:
                    for tq in range(4):
                        cs_ = slice(tq * 512, (tq + 1) * 512)
                        psY = k.ps[4 + tq]
                        t = [tc[(tcs["i"] + i) % 8] for i in range(3)]
                        tk = [("tc", (tcs["i"] + i) % 8) for i in range(3)]
                        tcs["i"] += 3
                        S("dve", lambda e, t=t, psY=psY, cs_=cs_: e.scalar_tensor_tensor(
                            t[0][:], ucT[:, cc, cs_], dsk[:, cc:cc + 1], psY[:], ALU.mult, ALU.add), ["ucT", "dsk", ("ps", 4 + tq)], [tk[0]])
                        S("act", lambda e, t=t: e.activation(t[1][:], t[0][:], AF.Square), [tk[0]], [tk[1]])
                        S("act", lambda e, t=t: e.activation(t[1][:], t[1][:], AF.Identity, bias=k.cone[:], scale=0.044715), [tk[1], "cone"], [tk[1]])
                        S("dve", lambda e, t=t: e.tensor_tensor(t[1][:], t[1][:], t[0][:], ALU.mult), [tk[1], tk[0]], [tk[1]])
                        S("act", lambda e, t=t: e.activation(t[2][:], t[1][:], AF.Sigmoid, scale=1.5957691216), [tk[1]], [tk[2]])
                        S("dve", lambda e, t=t, cs_=cs_: e.tensor_tensor(gel[:, cc, cs_], t[0][:], t[2][:], ALU.mult), [tk[0], tk[2]], [("gel", cc, tq)])

            tables_a(0)
            tables_b(0)
            for st in range(16):
                if st + 1 < 16:
                    tables_a(st + 1)
                forward(st)
                if st + 1 < 16:
                    tables_b(st + 1)
                scan_back(st)
            P.barrier()
        tci = 0
        tc = [sbt(f"tcg{i}", [128, 512], F32) for i in range(8)]
        wglu = sbt("wglu", [128, 4, 1024], BF16)
        P.dma("pool", wglu[:], k.w_glu[l].rearrange("(k p) c -> p k c", p=128), writes=["wglu"], semkey="wglu")
        yo = [sbt(f"yco{i}", [128, 512], BF16) for i in range(2)]
        it = 0
        for oc in range(4):
            for tq in range(4):
                cs_ = slice(tq * 512, (tq + 1) * 512)
                b = it % 2
                it += 1
                psV, psG = k.ps[b], k.ps[2 + b]
                for kk in range(4):
                    S("pe", lambda e, psV=psV, kk=kk, oc=oc, cs_=cs_: e.matmul(psV[:], wglu[:, kk, oc * 128:(oc + 1) * 128], gel[:, kk, cs_],
                                                                       start=(kk == 0), stop=(kk == 3)),
                      ["wglu", ("gel", kk, tq)], [("ps", b)])
                for kk in range(4):
                    S("pe", lambda e, psG=psG, kk=kk, oc=oc, cs_=cs_: e.matmul(psG[:], wglu[:, kk, 512 + oc * 128:512 + (oc + 1) * 128], gel[:, kk, cs_],
                                                                       start=(kk == 0), stop=(kk == 3)),
                      ["wglu", ("gel", kk, tq)], [("ps", 2 + b)])
                t = tc[tci % 8]
                tk = ("tc", tci % 8)
                tci += 1
                S("act", lambda e, t=t, psG=psG, oc=oc: e.activation(t[:], psG[:], AF.Sigmoid, bias=bglu[:, 4 + oc:5 + oc], scale=1.0),
                  [("ps", 2 + b), "bglu"], [tk])
                S("dve", lambda e, t=t, psV=psV, oc=oc, b=b: e.scalar_tensor_tensor(yo[b][:], psV[:], bglu[:, oc:oc + 1], t[:], ALU.add, ALU.mult),
                  [("ps", b), "bglu", tk], [("yco", b)])
                P.dma("sp", k.s_yc[oc * 128:(oc + 1) * 128, cs_], yo[b][:], reads=[("yco", b)], semkey=("yco", b))
        P.barrier()


class LNBufs:
    def __init__(self, k, es, moe, l):
        nc, P = k.nc, k.P
        sbt = lambda n, s, d: es.enter_context(SBT(nc, n, list(s), d))
        self.stats = sbt("ln_stats", [128, 12], F32)
        self.mv = sbt("ln_mv", [128, 2], F32)
        self.std = sbt("ln_std", [128, 1], F32)
        self.rstd = sbt("ln_rstd", [128, 1], F32)
        self.eps = sbt("ln_eps", [128, 1], F32)
        self.gam = sbt("ln_gam", [128, D], F32)
        self.bet = sbt("ln_bet", [128, D], F32)
        P.op("dve", lambda e: e.memset(self.eps[:], EPS), writes=["ln_eps"])
        self.moe = moe
        if moe:
            j = l // 2
            self.hT32 = sbt("hT32", [128, 8, 128], F32)
            self.rt32 = sbt("rt32", [128, 8, 8], F32)
            self.rbb = sbt("rbb", [128, 8], F32)
            self.lg = sbt("lg", [128, 8], F32)
            self.m8 = sbt("lm8", [128, 8], F32)
            self.sel = sbt("lsel", [128, 8], F32)
            self.nm = sbt("lnm", [128, 1], F32)
            self.ex = sbt("lex", [128, 8], F32)
            self.den = sbt("lden", [128, 1], F32)
            self.gate = sbt("lgate", [128, 8], F32)
            self.gTs = sbt("lgTs", [8, 128], F32)
            P.dma("sp", self.rt32[:], k.moe_router[j].rearrange("(c p) e -> p c e", p=128), writes=["rt32"], semkey="rt32")
            P.dma("sp", self.rbb[:], k.moe_router_b[j:j + 1, :].partition_broadcast(128), writes=["rbb"], semkey="rbb")

    def load_params(self, k, g_ap, b_ap):
        P = k.P
        P.dma("sp", self.gam[:], g_ap.partition_broadcast(128), writes=["ln_gam"], semkey="ln_gam")
        P.dma("sp", self.bet[:], b_ap.partition_broadcast(128), writes=["ln_bet"], semkey="ln_bet")


def ln_tile(k, B, s, skey, tt, dst, route=False, gb_eng="pool", part="all"):
    P = k.P
    S = lambda eng, fn, r, w: P.op(eng, fn, reads=r, writes=w)
    if part in ("all", "math"):
        _ln_math(k, B, s, skey, tt, dst, gb_eng)
    if part in ("all", "tr"):
        _ln_tr(k, B, s, skey, tt, route)


def _ln_math(k, B, s, skey, tt, dst, gb_eng):
    P = k.P
    S = lambda eng, fn, r, w: P.op(eng, fn, reads=r, writes=w)
    S("dve", lambda e: e.bn_stats(B.stats[:, 0:6], s[:, 0:512]), [skey], ["ln_stats"])
    S("dve", lambda e: e.bn_stats(B.stats[:, 6:12], s[:, 512:1024]), [skey], ["ln_stats"])
    S("dve", lambda e: e.bn_aggr(B.mv[:], B.stats[:]), ["ln_stats"], ["ln_mv"])
    S("act", lambda e: e.activation(B.std[:], B.mv[:, 1:2], AF.Sqrt, bias=B.eps[:], scale=1.0), ["ln_mv", "ln_eps"], ["ln_std"])
    S("dve", lambda e: e.reciprocal(B.rstd[:], B.std[:]), ["ln_std"], ["ln_rstd"])
    S("dve", lambda e: e.tensor_scalar(s[:], s[:], B.mv[:, 0:1], B.rstd[:, 0:1], ALU.subtract, ALU.mult), [skey, "ln_mv", "ln_rstd"], [skey])
    S(gb_eng, lambda e: e.tensor_tensor(s[:], s[:], B.gam[:], ALU.mult), [skey, "ln_gam"], [skey])
    S(gb_eng, lambda e: e.tensor_tensor(s[:], s[:], B.bet[:], ALU.add), [skey, "ln_bet"], [skey])
    P.dma("sp", dst, s[:], reads=[skey], semkey=("lnout", skey))


def _ln_tr(k, B, s, skey, tt, route):
    P = k.P
    S = lambda eng, fn, r, w: P.op(eng, fn, reads=r, writes=w)
    for half in range(2):
        ps = k.ps[6 + half]
        pk = ("ps", 6 + half)
        for j in range(4):
            dc = half * 4 + j
            P.op("pe", lambda e, ps=ps, j=j, dc=dc: e.transpose(ps[:, j * 128:(j + 1) * 128], s[:, dc * 128:(dc + 1) * 128], k.ident[:]),
                 reads=[skey, "ident"], writes=[pk], pe_chain=(j > 0))
        P.op("act", lambda e, ps=ps, half=half: e.activation(
            k.hT[:, half * 4:(half + 1) * 4, tt * 128:(tt + 1) * 128],
            ps[:].rearrange("p (j t) -> p j t", j=4), AF.Copy), reads=[pk], writes=[("hT", tt)])
        if route:
            P.op("dve", lambda e, ps=ps, half=half: e.tensor_copy(
                B.hT32[:, half * 4:(half + 1) * 4, :], ps[:].rearrange("p (j t) -> p j t", j=4)), reads=[pk], writes=["hT32"])
    if route:
        psL = k.ps[5]
        for dc in range(8):
            P.op("pe", lambda e, dc=dc: e.matmul(psL[:, 0:8], B.hT32[:, dc, :], B.rt32[:, dc, :], start=(dc == 0), stop=(dc == 7)),
                 reads=["hT32", "rt32"], writes=[("ps", 5)], pe_chain=(dc > 0))
        S("dve", lambda e: e.tensor_tensor(B.lg[:], psL[:, 0:8], B.rbb[:], ALU.add), [("ps", 5), "rbb"], ["lg"])
        S("dve", lambda e: e.max(B.m8[:], B.lg[:]), ["lg"], ["lm8"])
        S("dve", lambda e: e.tensor_scalar(B.sel[:], B.lg[:], B.m8[:, 1:2], None, ALU.is_ge), ["lg", "lm8"], ["lsel"])
        S("dve", lambda e: e.tensor_scalar(B.nm[:], B.m8[:, 0:1], -1.0, None, ALU.mult), ["lm8"], ["lnm"])
        S("act", lambda e: e.activation(B.ex[:], B.lg[:], AF.Exp, bias=B.nm[:], scale=1.0), ["lg", "lnm"], ["lex"])
        S("dve", lambda e: e.tensor_tensor(B.ex[:], B.ex[:], B.sel[:], ALU.mult), ["lex", "lsel"], ["lex"])
        S("dve", lambda e: e.reduce_sum(B.den[:], B.ex[:], AX.X), ["lex"], ["lden"])
        S("dve", lambda e: e.reciprocal(B.den[:], B.den[:]), ["lden"], ["lden"])
        S("dve", lambda e: e.tensor_scalar(B.gate[:], B.ex[:], B.den[:, 0:1], None, ALU.mult), ["lex", "lden"], ["lgate"])
        P.dma("sp", k.s_gate[tt * 128:(tt + 1) * 128, :], B.gate[:], reads=["lgate"], semkey="lgate")


def stage_merge(k, l):
    P, nc = k.P, k.nc
    moe = (l % 2 == 1)
    hsrc = k.x if l == 0 else k.s_h
    with ExitStack() as es:
        sbt = lambda n, s, d: es.enter_context(SBT(nc, n, list(s), d))
        wbr = sbt("wbr", [128, 12, 1024], BF16)
        wout = sbt("wout", [128, 8, 1024], BF16)
        ytq = [sbt(f"ytq{i}", [128, 12, 512], BF16) for i in range(2)]
        gtq2 = [sbt(f"gtq{i}", [128, 24, 512], BF16) for i in range(2)]
        mT = [sbt(f"mT{i}", [128, 8, 512], BF16) for i in range(2)]
        mt = [sbt(f"mtmp{i}", [128, 512], F32) for i in range(4)]
        hti = [sbt(f"hti{i}", [128, D], F32) for i in range(2)]
        st_ = [sbt(f"lns{i}", [128, D], F32) for i in range(2)]
        B = LNBufs(k, es, moe, l)
        B.load_params(k, k.ln1_g[l:l + 1, :], k.ln1_b[l:l + 1, :])
        P.dma("pool", wbr[:], k.w_branch[l].rearrange("(c p) d -> p c d", p=128), writes=["wbr"], semkey="wbr")
        P.dma("pool", wout[:], k.w_out[l].rearrange("(c p) d -> p c d", p=128), writes=["wout"], semkey="wout")
        ysrc = [k.s_ya, k.s_yb, k.s_yc]
        mi = 0
        ti = 0
        for tq in range(4):
            cs_ = slice(tq * 512, (tq + 1) * 512)
            yb = tq % 2
            for n in range(3):
                P.dma("sp", ytq[yb][:, n * 4:(n + 1) * 4, :], ysrc[n].rearrange("(c p) t -> p c t", p=128)[:, :, cs_],
                      writes=[("ytq", yb)], semkey=("ytq", yb))
            gtq = gtq2[tq % 2]
            gk = ("gtq", tq % 2)
            P.dma("sp", gtq[:], k.s_g.rearrange("(c p) t -> p c t", p=128)[:, :, cs_], writes=[gk], semkey=gk)
            mb = tq % 2
            for dc in range(8):
                for n in range(3):
                    for kk in range(4):
                        P.op("pe", lambda e, n=n, kk=kk, dc=dc, yb=yb: e.matmul(
                            k.ps[n][:], wbr[:, n * 4 + kk, dc * 128:(dc + 1) * 128], ytq[yb][:, n * 4 + kk, :],
                            start=(kk == 0), stop=(kk == 3)), reads=["wbr", ("ytq", yb)], writes=[("ps", n)], pe_chain=(kk > 0))
                t = [mt[(mi + i) % 4] for i in range(2)]
                tk = [("mtmp", (mi + i) % 4) for i in range(2)]
                mi += 2
                P.op("dve", lambda e, t=t, dc=dc, gtq=gtq: e.tensor_tensor(t[0][:], k.ps[0][:], gtq[:, dc, :], ALU.mult), reads=[("ps", 0), gk], writes=[tk[0]])
                P.op("dve", lambda e, t=t, dc=dc, gtq=gtq: e.tensor_tensor(t[1][:], k.ps[1][:], gtq[:, 8 + dc, :], ALU.mult), reads=[("ps", 1), gk], writes=[tk[1]])
                P.op("dve", lambda e, t=t: e.tensor_tensor(t[0][:], t[0][:], t[1][:], ALU.add), reads=[tk[0], tk[1]], writes=[tk[0]])
                P.op("dve", lambda e, t=t, dc=dc, gtq=gtq: e.tensor_tensor(t[1][:], k.ps[2][:], gtq[:, 16 + dc, :], ALU.mult), reads=[("ps", 2), gk], writes=[tk[1]])
                P.op("dve", lambda e, t=t, dc=dc, mb=mb: e.tensor_tensor(mT[mb][:, dc, :], t[0][:], t[1][:], ALU.add),
                     reads=[tk[0], tk[1]], writes=[("mT", mb)])
            for j in range(4):
                tt = tq * 4 + j
                b = ti % 2
                ti += 1
                P.dma("sp", hti[b][:], hsrc[tt * 128:(tt + 1) * 128, :], writes=[("hti", b)], semkey=("hti", b))
                for half in range(2):
                    psW = k.ps[3 + half]
                    for dc in range(8):
                        P.op("pe", lambda e, psW=psW, dc=dc, j=j, half=half, mb=mb: e.matmul(
                            psW[:], mT[mb][:, dc, j * 128:(j + 1) * 128], wout[:, dc, half * 512:(half + 1) * 512],
                            start=(dc == 0), stop=(dc == 7)), reads=[("mT", mb), "wout"], writes=[("ps", 3 + half)], pe_chain=(dc > 0))
                    P.op("dve", lambda e, psW=psW, b=b, half=half: e.scalar_tensor_tensor(
                        st_[b][:, half * 512:(half + 1) * 512], hti[b][:, half * 512:(half + 1) * 512], ALPHA, psW[:], ALU.mult, ALU.add),
                        reads=[("hti", b), ("ps", 3 + half)], writes=[("lns", b)])
                ln_tile(k, B, st_[b], ("lns", b), tt, k.s_h[tt * 128:(tt + 1) * 128, :], route=moe, gb_eng="dve")
        P.barrier()


def stage_ffn(k, l):
    P, nc = k.P, k.nc
    moe = (l % 2 == 1)
    jl = l // 2
    last = (l == 3)
    ne = 8 if moe else 1
    w1s = k.moe_w1 if moe else k.ffn_w1
    w3s = k.moe_w3 if moe else k.ffn_w3
    w2s = k.moe_w2 if moe else k.ffn_w2
    TH = 1024
    with ExitStack() as es:
        sbt = lambda n, s, d: es.enter_context(SBT(nc, n, list(s), d))
        w2sb = [sbt(f"w2sb{i}", [128, NF, 512], BF16) for i in range(2)]
        actT = sbt("actT", [128, NF, TH], BF16)
        w13 = [sbt(f"w13_{i}", [128, 2, 8, 256], BF16) for i in range(3)]
        sa = [sbt(f"sa{i}", [128, 512], F32) for i in range(3)]
        st_ = [sbt(f"flns{i}", [128, D], F32) for i in range(2)]
        facc = sbt("facc", [128, 8, D], F32)
        B = LNBufs(k, es, False, l)
        B.load_params(k, k.ln2_g[l:l + 1, :], k.ln2_b[l:l + 1, :])
        if moe:
            gsb = sbt("gsb", [128, NT, 8], F32)
            P.dma("sp", gsb[:], k.s_gate.rearrange("(t p) e -> p t e", p=128), writes=["gsb"], semkey="gsb")
        cnt = {"wi": 0, "ai": 0, "wq": 0}

        def up(th, ex, hook=None):
            t0 = th * TH
            ei = jl * 8 + ex if moe else jl
            w1v = w1s[ei].rearrange("(c p) f -> p c f", p=128)
            w3v = w3s[ei].rearrange("(c p) f -> p c f", p=128)
            w2v = w2s[ei].rearrange("(c p) d -> p c d", p=128)
            for fb in range(11):
                if hook is not None:
                    hook(fb)
                if fb == 3:
                    for hd in range(2):
                        P.dma("pool", w2sb[hd][:], w2v[:, :, hd * 512:(hd + 1) * 512], writes=[("w2sb", hd)], semkey=("w2sb", hd))
                wb_ = cnt["wi"] % 3
                cnt["wi"] += 1
                P.dma("pool", w13[wb_][:, 0, :, :], w1v[:, :, fb * 256:(fb + 1) * 256], writes=[("w13", wb_)], semkey=("w13", wb_))
                P.dma("pool", w13[wb_][:, 1, :, :], w3v[:, :, fb * 256:(fb + 1) * 256], writes=[("w13", wb_)], semkey=("w13", wb_))
                for jj in range(2):
                    fj = fb * 2 + jj
                    for c2 in range(2):
                        tcols = slice(t0 + c2 * 512, t0 + (c2 + 1) * 512)
                        pa = cnt["ai"] % 3
                        cnt["ai"] += 1
                        psA, psBm = k.ps[pa * 2], k.ps[pa * 2 + 1]
                        hk = [("hT", t0 // 128 + c2 * 4 + q) for q in range(4)]
                        for kk in range(8):
                            P.op("pe", lambda e, psA=psA, kk=kk, jj=jj, wb_=wb_, tcols=tcols: e.matmul(
                                psA[:], w13[wb_][:, 0, kk, jj * 128:(jj + 1) * 128], k.hT[:, kk, tcols], start=(kk == 0), stop=(kk == 7)),
                                reads=[("w13", wb_)] + hk, writes=[("ps", pa * 2)], pe_chain=(kk > 0))
                        for kk in range(8):
                            P.op("pe", lambda e, psBm=psBm, kk=kk, jj=jj, wb_=wb_, tcols=tcols: e.matmul(
                                psBm[:], w13[wb_][:, 1, kk, jj * 128:(jj + 1) * 128], k.hT[:, kk, tcols], start=(kk == 0), stop=(kk == 7)),
                                reads=[("w13", wb_)] + hk, writes=[("ps", pa * 2 + 1)], pe_chain=(kk > 0))
                        P.op("act", lambda e, psA=psA, pa=pa: e.activation(sa[pa][:], psA[:], AF.Silu), reads=[("ps", pa * 2)], writes=[("sa", pa)])
                        ocols = slice(c2 * 512, (c2 + 1) * 512)
                        P.op("dve", lambda e, psBm=psBm, pa=pa, fj=fj, ocols=ocols: e.tensor_tensor(actT[:, fj, ocols], psBm[:], sa[pa][:], ALU.mult),
                             reads=[("ps", pa * 2 + 1), ("sa", pa)], writes=[("actT", fj, c2)])

        def down(th, ex, hook=None):
            t0 = th * TH
            ei = jl * 8 + ex if moe else jl
            for tl in range(8):
                for hd in range(2):
                    tt = t0 // 128 + tl
                    psW = k.ps[6 + (cnt["wq"] % 2)]
                    pwk = ("ps", 6 + (cnt["wq"] % 2))
                    cnt["wq"] += 1
                    ak = [("actT", fj, tl // 4) for fj in range(NF)]
                    for fj in range(NF):
                        P.op("pe", lambda e, psW=psW, fj=fj, tl=tl, hd=hd: e.matmul(
                            psW[:], actT[:, fj, tl * 128:(tl + 1) * 128], w2sb[hd][:, fj, :], start=(fj == 0), stop=(fj == NF - 1)),
                            reads=[("w2sb", hd)] + (ak if fj == 0 else []), writes=[pwk], pe_chain=(fj > 0))
                    fk = ("facc", tl, hd)
                    fsl = facc[:, tl, hd * 512:(hd + 1) * 512]
                    if moe:
                        gap = gsb[:, tt, ex:ex + 1]
                        if ex == 0:
                            P.op("dve", lambda e, psW=psW, fsl=fsl, gap=gap: e.tensor_scalar(fsl, psW[:], gap, None, ALU.mult),
                                 reads=[pwk, "gsb"], writes=[fk])
                        else:
                            P.op("dve", lambda e, psW=psW, fsl=fsl, gap=gap: e.scalar_tensor_tensor(fsl, psW[:], gap, fsl, ALU.mult, ALU.add),
                                 reads=[pwk, "gsb", fk], writes=[fk])
                    else:
                        P.op("act", lambda e, psW=psW, fsl=fsl: e.activation(fsl, psW[:], AF.Copy), reads=[pwk], writes=[fk])
                if hook is not None:
                    hook(tl)

        def ln_part(th, tl, part):
            t0 = th * TH
            tt = t0 // 128 + tl
            b = tl % 2
            sk = ("flns", b)
            dst = (k.y if last else k.s_h)[tt * 128:(tt + 1) * 128, :]
            if part == "math":
                P.dma("sp", st_[b][:], k.s_h[tt * 128:(tt + 1) * 128, :], writes=[sk], semkey=("fhti", b))
                P.op("dve", lambda e: e.scalar_tensor_tensor(st_[b][:], st_[b][:], ALPHA, facc[:, tl, :], ALU.mult, ALU.add),
                     reads=[sk, ("facc", tl, 0), ("facc", tl, 1)], writes=[sk])
            ln_tile(k, B, st_[b], sk, tt, dst, route=False, gb_eng="dve", part=part)

        def hook0(fb):
            if 1 <= fb <= 8:
                ln_part(0, fb - 1, "math")
            if 2 <= fb <= 9:
                ln_part(0, fb - 2, "tr")

        for ex in range(ne):
            up(0, ex)
            down(0, ex)
        def hook1(tl):
            ln_part(1, tl, "math")
            if tl >= 1:
                ln_part(1, tl - 1, "tr")

        up(1, 0, hook0)
        down(1, 0, hook1 if ne == 1 else None)
        for ex in range(1, ne):
            up(1, ex)
            down(1, ex, hook1 if ex == ne - 1 else None)
        ln_part(1, 7, "tr")
        P.barrier()


def kernel(**inputs):
    inp = {k_: np.asarray(v) for k_, v in inputs.items()}
    lay = host_layout(inp)
    cst = host_consts()
    nc = build_program(4)
    base = dict(lay)
    base.update(cst)
    in_maps = []
    for b in range(8):
        m = dict(base)
        m["x"] = np.ascontiguousarray(inp["x"][b])
        in_maps.append(m)
    res = run_bass_kernel_spmd(nc, in_maps, core_ids=list(range(8)))
    out = np.stack([np.asarray(r["y"]) for r in res.results], axis=0).astype(np.float32)
    return out
```

```python
import concourse.bass as bass
import concourse.mybir as mybir

F32 = mybir.dt.float32
BF16 = mybir.dt.bfloat16
ALU = mybir.AluOpType
AF = mybir.ActivationFunctionType
AX = mybir.AxisListType

EPOCH = 30000


class Prog:
    def __init__(self, nc, es):
        self.nc = nc
        self.es = es
        self.eng = {"pe": nc.tensor, "act": nc.scalar, "dve": nc.vector,
                    "pool": nc.gpsimd, "sp": nc.sync}
        self.cur = {}
        self.waited = {e: {} for e in self.eng}
        self.last_w = {}
        self.rd = {}
        self.dsem = {}
        self.nsem = 0
        self.nins = {e: 0 for e in self.eng}
        self.all_events = []

    def _newsem(self, name):
        self.nsem += 1
        return self.es.enter_context(self.nc.semaphore(f"{name}_{self.nsem}"))

    def _engsem(self, e):
        c = self.cur.get(e)
        if c is None or c[1] >= EPOCH:
            c = [self._newsem("s" + e), 0]
            self.cur[e] = c
        return c

    def _deps(self, reads, writes):
        ev = []
        for k in reads:
            w = self.last_w.get(k)
            if w is not None:
                ev.append(w)
        for k in writes:
            w = self.last_w.get(k)
            if w is not None:
                ev.append(w)
            ev.extend(self.rd.get(k, ()))
        return ev

    def _emit_waits(self, e, evs, skip_self_sem=None):
        best = {}
        for (s, v) in evs:
            if skip_self_sem is not None and s.num == skip_self_sem.num:
                continue
            if v > best.get(s.num, (None, 0))[1]:
                best[s.num] = (s, v)
        wd = self.waited[e]
        for num, (s, v) in best.items():
            if wd.get(num, 0) >= v:
                continue
            self.eng[e].wait_ge(s, v)
            wd[num] = v

    def _record(self, ev, reads, writes):
        for k in writes:
            self.last_w[k] = ev
            self.rd[k] = []
        for k in reads:
            if k in writes:
                continue
            self.rd.setdefault(k, []).append(ev)

    def op(self, e, fn, reads=(), writes=(), pe_chain=False):
        psr = [k for k in reads if isinstance(k, tuple) and k[0] == "ps"]
        if psr:
            reads = [k for k in reads if k not in psr]
            writes = list(writes) + [k for k in psr if k not in writes]
        evs = self._deps(reads, writes)
        c = self._engsem(e)
        self._emit_waits(e, evs, skip_self_sem=c[0] if (e == "pe" and pe_chain) else None)
        ins = fn(self.eng[e])
        c[1] += 1
        ins.then_inc(c[0], 1)
        ev = (c[0], c[1])
        self._record(ev, reads, writes)
        self.nins[e] += 1
        return ev

    def dma(self, e, out, in_, reads=(), writes=(), semkey=None, **kw):
        evs = self._deps(reads, writes)
        self._emit_waits(e, evs)
        d = self.dsem.get(semkey)
        if d is None:
            d = [self._newsem("d"), 0]
            self.dsem[semkey] = d
        ins = self.eng[e].dma_start(out=out, in_=in_, **kw)
        d[1] += 16
        assert d[1] < 2 * EPOCH, f"dma sem overflow {semkey}"
        ins.then_inc(d[0], 16)
        ev = (d[0], d[1])
        self._record(ev, reads, writes)
        self.nins[e] += 1
        return ev

    def wait_all(self, e):
        evs = []
        for k, c in self.cur.items():
            if c[1] > 0:
                evs.append((c[0], c[1]))
        for k, d in self.dsem.items():
            if d[1] > 0:
                evs.append((d[0], d[1]))
        for k, w in self.last_w.items():
            evs.append(w)
        for k, r in self.rd.items():
            evs.extend(r)
        self._emit_waits(e, evs)

    def barrier(self):
        for e in self.eng:
            self.wait_all(e)
        self.last_w = {}
        self.rd = {}


import numpy as np
from contextlib import ExitStack
import concourse.bass as bass
import concourse.mybir as mybir
from concourse.bass_utils import run_bass_kernel_spmd

T = 2048
D = 1024
NT = 16
INW = 5632
DFF = 2816
NF = 22
ALPHA = 8 ** 0.25
EPS = 1e-5
NEGM = -240000.0
MAGIC = 12582912.0
TWO_PI_LO = 6.283185
PI_LO = 3.1415925


def host_consts():
    c = {}
    pw = np.zeros((4, 3, 128, 128), np.float32)
    for gi, w in enumerate((2, 4, 8, 16)):
        A = np.zeros((256, 256), np.float32)
        for t in range(256):
            st = max(t + 1 - w, 0)
            A[t, st:t + 1] = 1.0 / (t + 1 - st)
            A[t, t] -= 1.0
        pw[gi, 0] = A[0:128, 0:128].T
        pw[gi, 1] = A[128:256, 128:256].T
        pw[gi, 2] = A[128:256, 0:128].T
    c["c_pw"] = np.ascontiguousarray(pw.transpose(2, 0, 1, 3)).reshape(128, 4 * 3 * 128)
    ind = np.zeros((128, 64, 128), np.float32)
    for r in range(64):
        ind[r, r, :] = 1.0
    c["c_ind"] = ind.reshape(128, 64 * 128)
    slopes = 2.0 ** (-8.0 * np.arange(1, 9) / 8)
    eb = np.zeros((128, 8, 17), np.float32)
    ki = np.arange(128)
    for h in range(8):
        for m in range(17):
            eb[:, h, m] = slopes[h] * (ki - 128 * (m - 1) - 128)
    c["c_expb"] = eb.reshape(128, 136)
    cb = np.zeros((128, 128), np.float32)
    for k in range(128):
        cb[k, :k] = NEGM
    c["c_cbtri"] = cb
    nq = np.zeros((128, 8, 8, 8), np.float32)
    for qb in range(8):
        nq[:, qb, :, qb:] = -1e30
    c["c_negq"] = nq.reshape(128, 8 * 64)
    c["c_ident"] = np.eye(128, dtype=np.float32)
    c["c_iota"] = np.arange(T, dtype=np.float32).reshape(1, T)
    sel8 = np.zeros((8, 8, 128), np.float32)
    for e in range(8):
        sel8[e, e, :] = 1.0
    c["c_sel8"] = sel8.reshape(8, 8 * 128)
    return c


def host_layout(inp):
    o = {}
    L = 4
    o["w_in"] = inp["w_in"]
    o["b_in_fm"] = np.ascontiguousarray(inp["b_in"].reshape(L, 44, 128).transpose(0, 2, 1))
    o["b_in"] = inp["b_in"]
    o["w_pool"] = np.ascontiguousarray(inp["w_pool"].transpose(0, 2, 1, 3)).reshape(L, 128, 512)
    o["pool_scale"] = np.ascontiguousarray(inp["pool_scale"].reshape(L, 4, 128).transpose(0, 2, 1))

    def st_layout(a):
        return np.ascontiguousarray(a.reshape(L, 16, 2, 64).transpose(0, 2, 3, 1)).reshape(L, 128, 16)
    o["a_re"] = st_layout(inp["ssm_a_re"])
    o["a_im"] = st_layout(inp["ssm_a_im"])
    ld = np.repeat(inp["ssm_log_dt"].reshape(L, 16, 2, 1), 64, axis=3)
    o["log_dt"] = np.ascontiguousarray(ld.transpose(0, 2, 3, 1)).reshape(L, 128, 16)

    def b_layout(b):
        out = np.zeros((L, 128, 16, 128), np.float32)
        for g in range(32):
            st, half, gl = g // 2, g % 2, g % 8
            out[:, gl * 16:(gl + 1) * 16, st, half * 64:(half + 1) * 64] = b[:, g].transpose(0, 2, 1)
        return out.reshape(L, 128, 16 * 128)

    def c_layout(cm):
        out = np.zeros((L, 128, 16, 128), np.float32)
        for g in range(32):
            st, half, gl = g // 2, g % 2, g % 8
            out[:, half * 64:(half + 1) * 64, st, gl * 16:(gl + 1) * 16] = cm[:, g].transpose(0, 2, 1)
        return out.reshape(L, 128, 16 * 128)
    o["b_re"] = b_layout(inp["ssm_b_re"])
    o["b_im"] = b_layout(inp["ssm_b_im"])
    o["c_re"] = c_layout(inp["ssm_c_re"])
    o["c_im"] = c_layout(inp["ssm_c_im"])
    o["ssm_d"] = np.ascontiguousarray(inp["ssm_d"].reshape(L, 4, 128).transpose(0, 2, 1))
    o["w_glu"] = inp["w_glu"]
    o["b_glu"] = np.ascontiguousarray(inp["b_glu"].reshape(L, 8, 128).transpose(0, 2, 1))
    o["w_branch"] = inp["w_branch"].reshape(L, 1536, 1024)
    o["w_out"] = inp["w_out"]
    for n in ("ln1_g", "ln1_b", "ln2_g", "ln2_b"):
        o[n] = inp[n]
    o["ffn_w1"] = inp["ffn_w1"]
    o["ffn_w3"] = inp["ffn_w3"]
    o["ffn_w2"] = inp["ffn_w2"]
    o["moe_router"] = inp["moe_router"]
    o["moe_router_b"] = inp["moe_router_b"]
    o["moe_w1"] = inp["moe_w1"].reshape(16, 1024, DFF)
    o["moe_w3"] = inp["moe_w3"].reshape(16, 1024, DFF)
    o["moe_w2"] = inp["moe_w2"].reshape(16, DFF, 1024)
    return o


class K:
    pass


_U = [0]


def SBT(nc, name, shape, dt):
    _U[0] += 1
    return nc.sbuf_tensor(f"{name}_u{_U[0]}", list(shape), dt)


def build_program(n_layers=4, stop_after=None, dbg=()):
    nc = bass.Bass("TRN2", target_bir_lowering=False)
    k = K()
    k.nc = nc
    k.dbg = dbg

    def din(name, shape, dt=F32):
        return nc.dram_tensor(name, list(shape), dt, kind="ExternalInput").ap()

    def dscr(name, shape, dt):
        kind = "ExternalOutput" if name in dbg else "Internal"
        return nc.dram_tensor(name, list(shape), dt, kind=kind).ap()

    L = 4
    k.x = din("x", [T, D])
    k.y = nc.dram_tensor("y", [T, D], F32, kind="ExternalOutput").ap()
    k.w_in = din("w_in", [L, D, INW])
    k.b_in_fm = din("b_in_fm", [L, 128, 44])
    k.b_in = din("b_in", [L, INW])
    k.w_pool = din("w_pool", [L, 128, 512])
    k.pool_scale = din("pool_scale", [L, 128, 4])
    for n in ("a_re", "a_im", "log_dt"):
        setattr(k, n, din(n, [L, 128, 16]))
    for n in ("b_re", "b_im", "c_re", "c_im"):
        setattr(k, n, din(n, [L, 128, 2048]))
    k.ssm_d = din("ssm_d", [L, 128, 4])
    k.w_glu = din("w_glu", [L, 512, 1024])
    k.b_glu = din("b_glu", [L, 128, 8])
    k.w_branch = din("w_branch", [L, 1536, 1024])
    k.w_out = din("w_out", [L, D, D])
    for n in ("ln1_g", "ln1_b", "ln2_g", "ln2_b"):
        setattr(k, n, din(n, [L, D]))
    k.ffn_w1 = din("ffn_w1", [2, D, DFF])
    k.ffn_w3 = din("ffn_w3", [2, D, DFF])
    k.ffn_w2 = din("ffn_w2", [2, DFF, D])
    k.moe_router = din("moe_router", [2, D, 8])
    k.moe_router_b = din("moe_router_b", [2, 8])
    k.moe_w1 = din("moe_w1", [16, D, DFF])
    k.moe_w3 = din("moe_w3", [16, D, DFF])
    k.moe_w2 = din("moe_w2", [16, DFF, D])
    k.c_pw = din("c_pw", [128, 1536])
    k.c_ind = din("c_ind", [128, 8192])
    k.c_expb = din("c_expb", [128, 136])
    k.c_cbtri = din("c_cbtri", [128, 128])
    k.c_negq = din("c_negq", [128, 512])
    k.c_ident = din("c_ident", [128, 128])
    k.c_iota = din("c_iota", [1, T])
    k.c_sel8 = din("c_sel8", [8, 1024])
    k.s_h = dscr("s_h", [T, D], F32)
    k.s_ua = dscr("s_ua", [T, 512], BF16)
    k.s_v = dscr("s_v", [T, 512], BF16)
    k.s_q = dscr("s_q", [512, T], BF16)
    k.s_k = dscr("s_k", [512, T], BF16)
    k.s_uc = dscr("s_uc", [512, T], BF16)
    k.s_g = dscr("s_g", [3072, T], BF16)
    k.s_ya = dscr("s_ya", [512, T], BF16)
    k.s_yb = dscr("s_yb", [512, T], BF16)
    k.s_yc = dscr("s_yc", [512, T], BF16)
    k.s_gate = dscr("s_gate", [T, 8], F32)

    with ExitStack() as es:
        P = Prog(nc, es)
        k.P = P
        sbt = lambda n, s, d: es.enter_context(SBT(nc, n, list(s), d))
        k.ps = [es.enter_context(nc.psum_tensor(f"ps{i}", [128, 512], F32)) for i in range(8)]
        k.hT = sbt("hT", [128, 8, T], BF16)
        k.ident = sbt("ident", [128, 128], F32)
        k.identb = sbt("identb", [128, 128], BF16)
        k.cone = sbt("cone", [128, 1], F32)
        k.cnmag = sbt("cnmag", [128, 1], F32)
        k.cpmag = sbt("cpmag", [128, 1], F32)
        P.op("dve", lambda e: e.memset(k.cpmag[:], MAGIC), writes=["cpmag"])
        P.op("dve", lambda e: e.memset(k.cone[:], 1.0), writes=["cone"])
        P.op("dve", lambda e: e.memset(k.cnmag[:], -MAGIC), writes=["cnmag"])
        P.dma("sp", k.ident[:], k.c_ident, writes=["ident"], semkey="c0")
        P.dma("pool", k.identb[:], k.c_ident, writes=["identb"], semkey="c1")

        stage_init(k)
        for l in range(n_layers):
            stages = [("s1", stage_inproj), ("s2", stage_pool), ("s3", stage_attn), ("s4", stage_s5),
                      ("s6", stage_merge), ("s8", stage_ffn)]
            done = False
            for nm, fn in stages:
                P.barrier()
                fn(k, l)
                if stop_after == (l, nm):
                    done = True
                    break
            if done:
                break
        P.barrier()
        for e in ("sp", "act", "pool", "dve", "pe"):
            P.wait_all(e)
        print("instr counts", P.nins, "sems", P.nsem)
    return nc


def to_hT(k, src, src_key, tt):
    P = k.P
    for half in range(2):
        ps = k.ps[6 + half]
        pk = ("ps", 6 + half)
        for j in range(4):
            dc = half * 4 + j
            P.op("pe", lambda e, ps=ps, j=j, dc=dc: e.transpose(ps[:, j * 128:(j + 1) * 128], src[:, dc * 128:(dc + 1) * 128], k.ident[:]),
                 reads=[src_key, "ident"], writes=[pk], pe_chain=(j > 0))
        P.op("act", lambda e, ps=ps, half=half: e.activation(
            k.hT[:, half * 4:(half + 1) * 4, tt * 128:(tt + 1) * 128],
            ps[:].rearrange("p (j t) -> p j t", j=4), AF.Copy),
            reads=[pk], writes=[("hT", tt)])


def stage_init(k):
    P, nc = k.P, k.nc
    with ExitStack() as es:
        xt = [es.enter_context(SBT(nc, f"xt{i}", [128, D], F32)) for i in range(2)]
        for tt in range(NT):
            b = tt % 2
            P.dma("sp", xt[b][:], k.x[tt * 128:(tt + 1) * 128, :], writes=[("xt", b)], semkey=("xt", b))
            to_hT(k, xt[b], ("xt", b), tt)
        P.barrier()


def hT_keys():
    return [("hT", t) for t in range(NT)]


def stage_inproj(k, l):
    P, nc = k.P, k.nc
    with ExitStack() as es:
        sbt = lambda n, s, d: es.enter_context(SBT(nc, n, list(s), d))
        wb = [sbt(f"wblk{i}", [128, 8, 512], BF16) for i in range(3)]
        ot = [sbt(f"ot{i}", [128, 512], BF16) for i in range(4)]
        bfm = sbt("bfm", [128, 44], F32)
        bbc = sbt("bbc", [128, 2, 512], F32)
        P.dma("sp", bfm[:], k.b_in_fm[l], writes=["bfm"], semkey="bfm")
        P.dma("sp", bbc[:, 0, :], k.b_in[l:l + 1, 0:512].partition_broadcast(128), writes=["bbc"], semkey="bbc")
        P.dma("sp", bbc[:, 1, :], k.b_in[l:l + 1, 1536:2048].partition_broadcast(128), writes=["bbc"], semkey="bbc")
        wsrc = k.w_in[l].rearrange("(k p) c -> p k c", p=128)
        oi = 0
        pi = 0
        for blk in range(11):
            wbi = blk % 3
            P.dma("pool", wb[wbi][:], wsrc[:, :, blk * 512:(blk + 1) * 512], writes=[("wblk", wbi)], semkey=("wblk", wbi))
            if blk in (0, 3):
                dst = k.s_ua if blk == 0 else k.s_v
                bi = 0 if blk == 0 else 1
                for tt in range(NT):
                    ps = k.ps[pi % 6]
                    pk = ("ps", pi % 6)
                    pi += 1
                    for kk in range(8):
                        P.op("pe", lambda e, ps=ps, kk=kk, tt=tt, wbi=wbi: e.matmul(
                            ps[:], k.hT[:, kk, tt * 128:(tt + 1) * 128], wb[wbi][:, kk, :], start=(kk == 0), stop=(kk == 7)),
                            reads=[("hT", tt), ("wblk", wbi)], writes=[pk], pe_chain=(kk > 0))
                    o = oi % 4
                    oi += 1
                    P.op("dve", lambda e, ps=ps, o=o, bi=bi: e.tensor_tensor(ot[o][:], ps[:], bbc[:, bi, :], ALU.add),
                         reads=[pk, "bbc"], writes=[("ot", o)])
                    P.dma("sp", dst[tt * 128:(tt + 1) * 128, :], ot[o][:], reads=[("ot", o)], semkey=("ot", o))
            else:
                for c in range(4):
                    col = blk * 4 + c
                    if blk == 1:
                        dst, func = k.s_q[c * 128:(c + 1) * 128, :], AF.Identity
                    elif blk == 2:
                        dst, func = k.s_k[c * 128:(c + 1) * 128, :], AF.Identity
                    elif blk == 4:
                        dst, func = k.s_uc[c * 128:(c + 1) * 128, :], AF.Identity
                    else:
                        r0 = (blk - 5) * 512 + c * 128
                        dst, func = k.s_g[r0:r0 + 128, :], AF.Sigmoid
                    for tq in range(4):
                        ps = k.ps[pi % 6]
                        pk = ("ps", pi % 6)
                        pi += 1
                        for kk in range(8):
                            P.op("pe", lambda e, ps=ps, kk=kk, tq=tq, wbi=wbi, c=c: e.matmul(
                                ps[:], wb[wbi][:, kk, c * 128:(c + 1) * 128], k.hT[:, kk, tq * 512:(tq + 1) * 512],
                                start=(kk == 0), stop=(kk == 7)),
                                reads=[("hT", 4 * tq + j) for j in range(4)] + [("wblk", wbi)], writes=[pk], pe_chain=(kk > 0))
                        o = oi % 4
                        oi += 1
                        P.op("act", lambda e, ps=ps, o=o, col=col, func=func: e.activation(
                            ot[o][:], ps[:], func, bias=bfm[:, col:col + 1], scale=1.0),
                            reads=[pk, "bfm"], writes=[("ot", o)])
                        P.dma("sp", dst[:, tq * 512:(tq + 1) * 512], ot[o][:], reads=[("ot", o)], semkey=("ot", o))
        P.barrier()


def stage_pool(k, l):
    P, nc = k.P, k.nc
    with ExitStack() as es:
        sbt = lambda n, s, d: es.enter_context(SBT(nc, n, list(s), d))
        ua = sbt("ua", [128, NT, 512], BF16)
        pw = sbt("pw", [128, 4, 3, 128], BF16)
        wp = sbt("wp", [128, 4, 128], BF16)
        psc = sbt("psc", [128, 4], F32)
        zin = [sbt(f"zin{i}", [128, 512], BF16) for i in range(2)]
        yo = [sbt(f"yo{i}", [128, 512], BF16) for i in range(2)]
        P.dma("sp", ua[:], k.s_ua.rearrange("(t p) c -> p t c", p=128), writes=["ua"], semkey="ua")
        P.dma("pool", pw[:], k.c_pw.rearrange("p (g k t) -> p g k t", g=4, k=3), writes=["pw"], semkey="pw")
        P.dma("pool", wp[:], k.w_pool[l].rearrange("p (g d) -> p g d", g=4), writes=["wp"], semkey="wp")
        P.dma("sp", psc[:], k.pool_scale[l], writes=["psc"], semkey="psc")
        it = 0
        for g in range(4):
            for tq in range(4):
                b = it % 2
                it += 1
                psA, psB = k.ps[b], k.ps[2 + b]
                for j in range(4):
                    tt = tq * 4 + j
                    kind = 0 if tt == 0 else 1
                    P.op("pe", lambda e, psA=psA, j=j, tt=tt, g=g, kind=kind: e.matmul(
                        psA[:, j * 128:(j + 1) * 128], ua[:, tt, g * 128:(g + 1) * 128], pw[:, g, kind, :],
                        start=True, stop=(tt == 0)), reads=["ua", "pw"], writes=[("ps", b)], pe_chain=(j > 0))
                    if tt > 0:
                        P.op("pe", lambda e, psA=psA, j=j, tt=tt, g=g: e.matmul(
                            psA[:, j * 128:(j + 1) * 128], ua[:, tt - 1, g * 128:(g + 1) * 128], pw[:, g, 2, :],
                            start=False, stop=True), reads=["ua", "pw"], writes=[("ps", b)], pe_chain=True)
                P.op("act", lambda e, psA=psA, b=b: e.activation(zin[b][:], psA[:], AF.Copy),
                     reads=[("ps", b)], writes=[("zin", b)])
                P.op("pe", lambda e, psB=psB, b=b, g=g: e.matmul(psB[:], wp[:, g, :], zin[b][:], start=True, stop=True),
                     reads=["wp", ("zin", b)], writes=[("ps", 2 + b)])
                P.op("act", lambda e, psB=psB, b=b, g=g: e.activation(yo[b][:], psB[:], AF.Copy, scale=psc[:, g:g + 1]),
                     reads=[("ps", 2 + b), "psc"], writes=[("yo", b)])
                P.dma("sp", k.s_ya[g * 128:(g + 1) * 128, tq * 512:(tq + 1) * 512], yo[b][:], reads=[("yo", b)], semkey=("yo", b))
        P.barrier()


def stage_attn(k, l):
    P, nc = k.P, k.nc
    with ExitStack() as es:
        sbt = lambda n, s, d: es.enter_context(SBT(nc, n, list(s), d))
        qT = sbt("qT", [128, 4, T], BF16)
        kT = sbt("kT", [128, 4, T], BF16)
        va = sbt("va", [128, NT, 8, 65], BF16)
        ind = sbt("ind", [128, 64, 128], BF16)
        expb = sbt("expb", [128, 8, 17], F32)
        cbt = sbt("cbt", [128, 128], BF16)
        negq = sbt("negq", [128, 8, 64], F32)
        ksum = sbt("ksum", [128, 4, 8], F32)
        kmT = sbt("kmT", [128, 4, 8], BF16)
        mbT = sbt("mbT", [128, T], BF16)
        gm = [sbt(f"gm{i}", [128, 64], F32) for i in range(2)]
        m8 = [sbt(f"m8{i}", [128, 8], F32) for i in range(2)]
        selb = [sbt(f"selb{i}", [128, 64], F32) for i in range(2)]
        pT = [sbt(f"pT{i}", [128, 256], BF16) for i in range(3)]
        rec = [sbt(f"rec{i}", [128, 1], F32) for i in range(4)]
        ybt = [sbt(f"ybt{i}", [128, 2, 512], BF16) for i in range(2)]
        ybo = [sbt(f"ybo{i}", [128, 4, 256], BF16) for i in range(2)]
        P.dma("sp", qT[:], k.s_q.rearrange("(c p) t -> p c t", p=128), writes=["qT"], semkey="qT")
        P.dma("sp", kT[:], k.s_k.rearrange("(c p) t -> p c t", p=128), writes=["kT"], semkey="kT")
        P.op("dve", lambda e: e.memset(va[:], 1.0), writes=["va"])
        P.op("dve", lambda e: e.memset(mbT[:], 0.0), writes=["mbT"])
        for tt in range(NT):
            P.dma("sp", va[:, tt, :, 0:64], k.s_v[tt * 128:(tt + 1) * 128, :].rearrange("p (h d) -> p h d", h=8),
                  writes=["va"], semkey="va")
        P.dma("pool", ind[:], k.c_ind.rearrange("p (j t) -> p j t", j=64), writes=["ind"], semkey="ind")
        P.dma("sp", expb[:], k.c_expb.rearrange("p (h m) -> p h m", h=8), writes=["expb"], semkey="expb")
        P.dma("pool", cbt[:], k.c_cbtri, writes=["cbt"], semkey="cbt")
        P.dma("sp", negq[:], k.c_negq.rearrange("p (q c) -> p q c", q=8), writes=["negq"], semkey="negq")
        ph = 9
        P.op("dve", lambda e: e.tensor_reduce(ksum[:], kT[:].rearrange("p c (n s) -> p c n s", s=256), AX.X, ALU.add),
             reads=["kT"], writes=["ksum"])
        P.op("act", lambda e: e.activation(kmT[:], ksum[:], AF.Copy, scale=1.0 / 256), reads=["ksum"], writes=["kmT"])
        psG = k.ps[7]
        for tt in range(8, NT):
            qb = tt // 2
            b = tt % 2
            for h in range(8):
                c, base = h // 2, (h % 2) * 64
                P.op("pe", lambda e, h=h, c=c, base=base, tt=tt: e.matmul(
                    psG[:, h * 8:(h + 1) * 8], qT[base:base + 64, c, tt * 128:(tt + 1) * 128], kmT[base:base + 64, c, :],
                    start=True, stop=True), reads=["qT", "kmT"], writes=[("ps", 7)])
            P.op("dve", lambda e, b=b, qb=qb: e.tensor_tensor(gm[b][:], psG[:, 0:64], negq[:, qb, :], ALU.add),
                 reads=[("ps", 7), "negq"], writes=[("gm", b)])
            for h in range(8):
                P.op("dve", lambda e, b=b, h=h: e.max(m8[b][:], gm[b][:, h * 8:(h + 1) * 8]),
                     reads=[("gm", b)], writes=[("m8", b)])
                P.op("dve", lambda e, b=b, h=h: e.tensor_scalar(selb[b][:, h * 8:(h + 1) * 8], gm[b][:, h * 8:(h + 1) * 8],
                                                               m8[b][:, 2:3], NEGM, ALU.is_lt, ALU.mult),
                     reads=[("gm", b), ("m8", b)], writes=[("selb", b)])
            P.op("pe", lambda e, b=b: e.transpose(psG[0:64, 128:256], selb[b][:], k.ident[:]),
                 reads=[("selb", b), "ident"], writes=[("ps", 7)])
            P.op("act", lambda e, tt=tt: e.activation(mbT[0:64, tt * 128:(tt + 1) * 128], psG[0:64, 128:256], AF.Copy),
                 reads=[("ps", 7)], writes=["mbT"])
        if ph == 1:
            P.barrier()
            return
        psTb = k.ps[3][:].bitcast(BF16)

        def make_unit(qb, h, kt, sb_, pb, ris):
            Q0 = qb * 256
            c, base = h // 2, (h % 2) * 64
            ob = h % 2
            psO = [k.ps[4 + 2 * ob], k.ps[5 + 2 * ob]]
            pok = [("ps", 4 + 2 * ob), ("ps", 5 + 2 * ob)]
            psS = k.ps[sb_]
            psk = ("ps", sb_)
            own_a = kt == 2 * qb
            own_b = kt == 2 * qb + 1
            kcols = slice(kt * 128, (kt + 1) * 128)
            js = [1] if own_b else [0, 1]
            yb = ybt[qb % 2]
            ybk = ("ybt", qb % 2)

            def score():
                if own_b:
                    P.op("pe", lambda e: e.matmul(
                        psS[:, 128:256], kT[base:base + 64, c, kcols], qT[base:base + 64, c, Q0 + 128:Q0 + 256],
                        start=True, stop=False), reads=["kT", "qT"], writes=[psk])
                    P.op("pe", lambda e: e.matmul(psS[:, 128:256], k.identb[:], cbt[:], start=False, stop=True),
                         reads=["identb", "cbt"], writes=[psk], pe_chain=True)
                elif own_a:
                    P.op("pe", lambda e: e.matmul(
                        psS[:, 0:128], kT[base:base + 64, c, kcols], qT[base:base + 64, c, Q0:Q0 + 128],
                        start=True, stop=False), reads=["kT", "qT"], writes=[psk])
                    P.op("pe", lambda e: e.matmul(psS[:, 0:128], k.identb[:], cbt[:], start=False, stop=True),
                         reads=["identb", "cbt"], writes=[psk], pe_chain=True)
                    P.op("pe", lambda e: e.matmul(
                        psS[:, 128:256], kT[base:base + 64, c, kcols], qT[base:base + 64, c, Q0 + 128:Q0 + 256],
                        start=True, stop=True), reads=["kT", "qT"], writes=[psk], pe_chain=True)
                else:
                    masked = qb >= 4
                    P.op("pe", lambda e: e.matmul(
                        psS[:, 0:256], kT[base:base + 64, c, kcols], qT[base:base + 64, c, Q0:Q0 + 256],
                        start=True, stop=(not masked)), reads=["kT", "qT"], writes=[psk])
                    if masked:
                        n = kt // 2
                        P.op("pe", lambda e: e.matmul(
                            psS[:, 0:256], ind[:, h * 8 + n, :], mbT[:, Q0:Q0 + 256], start=False, stop=True),
                            reads=["ind", "mbT"], writes=[psk], pe_chain=True)

            def exps():
                m = 2 * qb - kt + 1
                c0, c1 = js[0] * 128, (js[-1] + 1) * 128
                P.op("act", lambda e: e.activation(
                    pT[pb][:, c0:c1], psS[:, c0:c1], AF.Exp, bias=expb[:, h, m:m + 1], scale=0.125),
                    reads=[psk, "expb"], writes=[("pT", pb, j) for j in js])

            def pv():
                for j in js:
                    last = (kt == 2 * qb) if j == 0 else (kt == 2 * qb + 1)
                    P.op("pe", lambda e, j=j, last=last: e.matmul(
                        psO[j][:, 0:65], pT[pb][:, j * 128:(j + 1) * 128], va[:, kt, h, :], start=(kt == 0), stop=last),
                        reads=[("pT", pb, j), "va"], writes=[pok[j]], pe_chain=(kt > 0))
                if not own_b:
                    return
                for j in range(2):
                    r = ris[j]
                    P.op("dve", lambda e, r=r, j=j: e.reciprocal(rec[r][:], psO[j][:, 64:65]),
                         reads=[pok[j]], writes=[("rec", r)])
                    P.op("dve", lambda e, r=r, j=j: e.tensor_scalar(
                        yb[:, j, h * 64:(h + 1) * 64], psO[j][:, 0:64], rec[r][:, 0:1], None, ALU.mult),
                        reads=[pok[j], ("rec", r)], writes=[ybk])
                if h != 7:
                    return
                yo = ybo[qb % 2]
                yok = ("ybo", qb % 2)
                for j in range(2):
                    for cc in range(4):
                        P.op("pe", lambda e, j=j, cc=cc: e.transpose(
                            psTb[:, (j * 4 + cc) * 128:(j * 4 + cc + 1) * 128], yb[:, j, cc * 128:(cc + 1) * 128], k.identb[:]),
                            reads=[ybk, "identb"], writes=[("ps", 3)], pe_chain=(j + cc > 0))
                for j in range(2):
                    P.op("act", lambda e, j=j: e.activation(
                        yo[:, :, j * 128:(j + 1) * 128], psTb[:, j * 512:(j + 1) * 512].rearrange("p (c t) -> p c t", c=4), AF.Copy),
                        reads=[("ps", 3)], writes=[yok])
                P.dma("sp", k.s_yb.rearrange("(c p) t -> p c t", p=128)[:, :, Q0:Q0 + 256], yo[:], reads=[yok], semkey=yok)
            return score, exps, pv

        units = []
        si = 0
        ri = 0
        for qb in range(8):
            for h in range(8):
                for kt in range(2 * qb + 2):
                    ris = (ri % 4, (ri + 1) % 4)
                    if kt == 2 * qb + 1:
                        ri += 2
                    units.append(make_unit(qb, h, kt, si % 3, si % 3, ris))
                    si += 1
        LAG = 1
        for i, u in enumerate(units):
            u[0]()
            u[1]()
            if i >= LAG:
                units[i - LAG][2]()
        for i in range(max(0, len(units) - LAG), len(units)):
            units[i][2]()
        P.barrier()


def stage_s5(k, l):
    P, nc = k.P, k.nc
    INV2PI = 0.15915494309189535
    with ExitStack() as es:
        sbt = lambda n, s, d: es.enter_context(SBT(nc, n, list(s), d))
        ucT = sbt("ucT", [128, 4, T], BF16)
        blr = sbt("blr", [128, 16, 128], BF16)
        bli = sbt("bli", [128, 16, 128], BF16)
        cpr = sbt("cpr", [128, 16, 128], F32)
        cpi = sbt("cpi", [128, 16, 128], F32)
        iota = sbt("iota", [128, T], F32)
        bglu = sbt("bglu", [128, 8], F32)
        dsk = sbt("dsk", [128, 4], F32)
        gel = sbt("gel", [128, 4, T], BF16)
        sm = {}
        for n in ("ar", "ai", "ldt", "dt", "ad", "mag", "th", "u", "kk", "thf", "sn", "sh", "cs", "lr", "li",
                  "den", "t1", "t2", "rden", "nr", "fr", "fi", "nfr"):
            sm[n] = sbt("sm_" + n, [128, 16], F32)
        P.dma("sp", ucT[:], k.s_uc.rearrange("(c p) t -> p c t", p=128), writes=["ucT"], semkey="ucT")
        P.dma("pool", blr[:], k.b_re[l].rearrange("p (s n) -> p s n", s=16), writes=["blr"], semkey="blr")
        P.dma("pool", bli[:], k.b_im[l].rearrange("p (s n) -> p s n", s=16), writes=["bli"], semkey="bli")
        P.dma("sp", iota[:], k.c_iota.partition_broadcast(128), writes=["iota"], semkey="iota")
        P.dma("sp", bglu[:], k.b_glu[l], writes=["bglu"], semkey="bglu")
        P.dma("sp", dsk[:], k.ssm_d[l], writes=["dsk"], semkey="dsk")
        P.dma("sp", sm["ar"][:], k.a_re[l], writes=["ar"], semkey="ar")
        P.dma("sp", sm["ai"][:], k.a_im[l], writes=["ai"], semkey="ai")
        P.dma("sp", sm["ldt"][:], k.log_dt[l], writes=["ldt"], semkey="ldt")

        def S(eng, fn, r, w):
            P.op(eng, fn, reads=r, writes=w)
        a = lambda n: sm[n][:]
        S("act", lambda e: e.activation(a("dt"), a("ldt"), AF.Exp), ["ldt"], ["dt"])
        S("dve", lambda e: e.tensor_tensor(a("ad"), a("ar"), a("dt"), ALU.mult), ["ar", "dt"], ["ad"])
        S("act", lambda e: e.activation(a("mag"), a("ad"), AF.Exp), ["ad"], ["mag"])
        S("dve", lambda e: e.tensor_tensor(a("th"), a("ai"), a("dt"), ALU.mult), ["ai", "dt"], ["th"])
        S("dve", lambda e: e.tensor_scalar(a("u"), a("th"), INV2PI, MAGIC, ALU.mult, ALU.add), ["th"], ["u"])
        S("dve", lambda e: e.tensor_scalar(a("kk"), a("u"), MAGIC, None, ALU.subtract), ["u"], ["kk"])
        S("dve", lambda e: e.scalar_tensor_tensor(a("thf"), a("th"), INV2PI, a("kk"), ALU.mult, ALU.subtract), ["th", "kk"], ["thf"])
        S("act", lambda e: e.activation(a("sn"), a("thf"), AF.Sin, scale=TWO_PI_LO), ["thf"], ["sn"])
        S("act", lambda e: e.activation(a("sh"), a("thf"), AF.Sin, scale=PI_LO), ["thf"], ["sh"])
        S("dve", lambda e: e.tensor_tensor(a("cs"), a("sh"), a("sh"), ALU.mult), ["sh"], ["cs"])
        S("dve", lambda e: e.tensor_scalar(a("cs"), a("cs"), -2.0, 1.0, ALU.mult, ALU.add), ["cs"], ["cs"])
        S("dve", lambda e: e.tensor_tensor(a("lr"), a("mag"), a("cs"), ALU.mult), ["mag", "cs"], ["lr"])
        S("dve", lambda e: e.tensor_tensor(a("li"), a("mag"), a("sn"), ALU.mult), ["mag", "sn"], ["li"])
        S("dve", lambda e: e.tensor_tensor(a("den"), a("ar"), a("ar"), ALU.mult), ["ar"], ["den"])
        S("dve", lambda e: e.tensor_tensor(a("t1"), a("ai"), a("ai"), ALU.mult), ["ai"], ["t1"])
        S("dve", lambda e: e.tensor_tensor(a("den"), a("den"), a("t1"), ALU.add), ["den", "t1"], ["den"])
        S("dve", lambda e: e.reciprocal(a("rden"), a("den")), ["den"], ["rden"])
        S("dve", lambda e: e.tensor_scalar(a("nr"), a("lr"), -1.0, None, ALU.add), ["lr"], ["nr"])
        S("dve", lambda e: e.tensor_tensor(a("t1"), a("nr"), a("ar"), ALU.mult), ["nr", "ar"], ["t1"])
        S("dve", lambda e: e.tensor_tensor(a("t2"), a("li"), a("ai"), ALU.mult), ["li", "ai"], ["t2"])
        S("dve", lambda e: e.tensor_tensor(a("t1"), a("t1"), a("t2"), ALU.add), ["t1", "t2"], ["t1"])
        S("dve", lambda e: e.tensor_tensor(a("fr"), a("t1"), a("rden"), ALU.mult), ["t1", "rden"], ["fr"])
        S("dve", lambda e: e.tensor_tensor(a("t1"), a("li"), a("ar"), ALU.mult), ["li", "ar"], ["t1"])
        S("dve", lambda e: e.tensor_tensor(a("t2"), a("nr"), a("ai"), ALU.mult), ["nr", "ai"], ["t2"])
        S("dve", lambda e: e.tensor_tensor(a("t1"), a("t1"), a("t2"), ALU.subtract), ["t1", "t2"], ["t1"])
        S("dve", lambda e: e.tensor_tensor(a("fi"), a("t1"), a("rden"), ALU.mult), ["t1", "rden"], ["fi"])
        S("dve", lambda e: e.tensor_scalar(a("nfr"), a("fr"), -1.0, None, ALU.mult), ["fr"], ["nfr"])
        with ExitStack() as es2:
            clr = es2.enter_context(SBT(nc, "clr", [128, 16, 128], F32))
            cli = es2.enter_context(SBT(nc, "cli", [128, 16, 128], F32))
            ctm = es2.enter_context(SBT(nc, "ctm", [128, 16, 128], F32))
            P.dma("sp", clr[:], k.c_re[l].rearrange("p (s n) -> p s n", s=16), writes=["clr"], semkey="clr")
            P.dma("sp", cli[:], k.c_im[l].rearrange("p (s n) -> p s n", s=16), writes=["cli"], semkey="cli")
            for st in range(16):
                fr_, fi_, nfr_ = sm["fr"][:, st:st + 1], sm["fi"][:, st:st + 1], sm["nfr"][:, st:st + 1]
                S("dve", lambda e, st=st, fi_=fi_: e.tensor_scalar(ctm[:, st, :], cli[:, st, :], fi_, None, ALU.mult),
                  ["cli", "fi"], [("ctm", st)])
                S("dve", lambda e, st=st, fr_=fr_: e.scalar_tensor_tensor(cpr[:, st, :], clr[:, st, :], fr_, ctm[:, st, :], ALU.mult, ALU.subtract),
                  ["clr", "fr", ("ctm", st)], [("cpr", st)])
                S("dve", lambda e, st=st, fi_=fi_: e.tensor_scalar(ctm[:, st, :], clr[:, st, :], fi_, None, ALU.mult),
                  ["clr", "fi"], [("ctm", st)])
                S("dve", lambda e, st=st, nfr_=nfr_: e.scalar_tensor_tensor(cpi[:, st, :], cli[:, st, :], nfr_, ctm[:, st, :], ALU.mult, ALU.subtract),
                  ["cli", "nfr", ("ctm", st)], [("cpi", st)])
            P.barrier()
        with ExitStack() as es3:
            sbt3 = lambda n, s, d: es3.enter_context(SBT(nc, n, list(s), d))
            big = {}
            for n in ("A", "B", "FR", "XR", "XI", "GR", "GI"):
                big[n] = sbt3("big_" + n, [128, T], F32)
            SN2 = [sbt3(f"big_SN{i}", [128, T], F32) for i in range(2)]
            CS2 = [sbt3(f"big_CS{i}", [128, T], F32) for i in range(2)]
            tc = [sbt3(f"tc{i}", [128, 512], F32) for i in range(8)]
            g = lambda n: big[n][:]
            tcs = {"i": 0}
            xrk = [("XR", i) for i in range(4)]
            xik = [("XI", i) for i in range(4)]

            def tables_a(st):
                thf_ = sm["thf"][:, st:st + 1]
                S("act", lambda e: e.activation(g("A"), iota[:], AF.Identity, bias=k.cpmag[:], scale=thf_), ["iota", "thf", "cpmag"], ["A"])
                S("act", lambda e: e.activation(g("B"), g("A"), AF.Identity, bias=k.cnmag[:], scale=1.0), ["A", "cnmag"], ["B"])

            def tables_b(st):
                p = st % 2
                thf_ = sm["thf"][:, st:st + 1]
                sn, cs = SN2[p], CS2[p]
                S("dve", lambda e: e.scalar_tensor_tensor(g("FR"), iota[:], thf_, g("B"), ALU.mult, ALU.subtract), ["iota", "thf", "B"], ["FR"])
                S("act", lambda e: e.activation(sn[:], g("FR"), AF.Sin, scale=TWO_PI_LO), ["FR"], [("SN", p)])
                S("act", lambda e: e.activation(cs[:], g("FR"), AF.Sin, scale=PI_LO), ["FR"], [("CS", p)])
                S("act", lambda e: e.activation(cs[:], cs[:], AF.Square), [("CS", p)], [("CS", p)])
                S("act", lambda e: e.activation(cs[:], cs[:], AF.Identity, bias=k.cone[:], scale=-2.0), [("CS", p), "cone"], [("CS", p)])

            def forward(st):
                cc = st // 4
                p = st % 2
                sn, cs = SN2[p], CS2[p]
                for tq in range(4):
                    cs_ = slice(tq * 512, (tq + 1) * 512)
                    pb = (tq % 2) * 2
                    psXr, psXi = k.ps[pb], k.ps[pb + 1]
                    S("pe", lambda e, psXr=psXr, cs_=cs_: e.matmul(psXr[:], blr[:, st, :], ucT[:, cc, cs_], start=True, stop=True),
                      ["blr", "ucT"], [("ps", pb)])
                    S("pe", lambda e, psXi=psXi, cs_=cs_: e.matmul(psXi[:], bli[:, st, :], ucT[:, cc, cs_], start=True, stop=True),
                      ["bli", "ucT"], [("ps", pb + 1)])
                    t = [tc[(tcs["i"] + i) % 8] for i in range(4)]
                    tk = [("tc", (tcs["i"] + i) % 8) for i in range(4)]
                    tcs["i"] += 4
                    S("dve", lambda e, t=t, psXr=psXr, cs_=cs_: e.tensor_tensor(t[0][:], psXr[:], cs[:, cs_], ALU.mult), [("ps", pb), ("CS", p)], [tk[0]])
                    S("dve", lambda e, t=t, psXi=psXi, cs_=cs_: e.tensor_tensor(t[1][:], psXi[:], sn[:, cs_], ALU.mult), [("ps", pb + 1), ("SN", p)], [tk[1]])
                    S("pool", lambda e, t=t, cs_=cs_: e.tensor_tensor(big["XR"][:, cs_], t[0][:], t[1][:], ALU.add), [tk[0], tk[1]], [("XR", tq)])
                    S("dve", lambda e, t=t, psXi=psXi, cs_=cs_: e.tensor_tensor(t[2][:], psXi[:], cs[:, cs_], ALU.mult), [("ps", pb + 1), ("CS", p)], [tk[2]])
                    S("dve", lambda e, t=t, psXr=psXr, cs_=cs_: e.tensor_tensor(t[3][:], psXr[:], sn[:, cs_], ALU.mult), [("ps", pb), ("SN", p)], [tk[3]])
                    S("pool", lambda e, t=t, cs_=cs_: e.tensor_tensor(big["XI"][:, cs_], t[2][:], t[3][:], ALU.subtract), [tk[2], tk[3]], [("XI", tq)])

            def scan_back(st):
                cc = st // 4
                p = st % 2
                sn, cs = SN2[p], CS2[p]
                snk, csk = ("SN", p), ("CS", p)
                rb = sm["mag"][:, st:st + 1].to_broadcast([128, T])
                S("dve", lambda e: e.tensor_tensor_scan(g("GR"), rb, g("XR"), 0.0, ALU.mult, ALU.add), ["mag"] + xrk, ["GR"])
                S("dve", lambda e: e.tensor_tensor_scan(g("GI"), rb, g("XI"), 0.0, ALU.mult, ALU.add), ["mag"] + xik, ["GI"])
                S("dve", lambda e: e.tensor_tensor(g("A"), g("GR"), cs[:], ALU.mult), ["GR", csk], ["A"])
                S("dve", lambda e: e.tensor_tensor(g("B"), g("GI"), sn[:], ALU.mult), ["GI", snk], ["B"])
                S("dve", lambda e: e.tensor_tensor(g("XR"), g("A"), g("B"), ALU.subtract), ["A", "B"], xrk)
                S("dve", lambda e: e.tensor_tensor(g("FR"), g("GR"), sn[:], ALU.mult), ["GR", snk], ["FR"])
                S("dve", lambda e: e.tensor_tensor(g("A"), g("GI"), cs[:], ALU.mult), ["GI", csk], ["A"])
                S("dve", lambda e: e.tensor_tensor(g("XI"), g("FR"), g("A"), ALU.add), ["FR", "A"], xik)
                for tq in range(4):
                    cs_ = slice(tq * 512, (tq + 1) * 512)
                    psY = k.ps[4 + tq]
                    S("pe", lambda e, psY=psY, cs_=cs_: e.matmul(psY[:], cpr[:, st, :], big["XR"][:, cs_], start=(st % 4 == 0), stop=False),
                      [("cpr", st)] + xrk, [("ps", 4 + tq)])
                    S("pe", lambda e, psY=psY, cs_=cs_: e.matmul(psY[:], cpi[:, st, :], big["XI"][:, cs_], start=False, stop=(st % 4 == 3)),
                      [("cpi", st)] + xik, [("ps", 4 + tq)])
                if st % 4 == 3:
                    for tq in range(4):
                        cs_ = slice(tq * 512, (tq + 1) * 512)
                        psY = k.ps[4 + tq]
                        t = [tc[(tcs["i"] + i) % 8] for i in range(3)]
                        tk = [("tc", (tcs["i"] + i) % 8) for i in range(3)]
                        tcs["i"] += 3
                        S("dve", lambda e, t=t, psY=psY, cs_=cs_: e.scalar_tensor_tensor(
                            t[0][:], ucT[:, cc, cs_], dsk[:, cc:cc + 1], psY[:], ALU.mult, ALU.add), ["ucT", "dsk", ("ps", 4 + tq)], [tk[0]])
                        S("act", lambda e, t=t: e.activation(t[1][:], t[0][:], AF.Square), [tk[0]], [tk[1]])
                        S("act", lambda e, t=t: e.activation(t[1][:], t[1][:], AF.Identity, bias=k.cone[:], scale=0.044715), [tk[1], "cone"], [tk[1]])
                        S("dve", lambda e, t=t: e.tensor_tensor(t[1][:], t[1][:], t[0][:], ALU.mult), [tk[1], tk[0]], [tk[1]])
                        S("act", lambda e, t=t: e.activation(t[2][:], t[1][:], AF.Sigmoid, scale=1.5957691216), [tk[1]], [tk[2]])
                        S("dve", lambda e, t=t, cs_=cs_: e.tensor_tensor(gel[:, cc, cs_], t[0][:], t[2][:], ALU.mult), [tk[0], tk[2]], [("gel", cc, tq)])

            tables_a(0)
            tables_b(0)
            for st in range(16):
                if st + 1 < 16:
                    tables_a(st + 1)
                forward(st)
                if st + 1 < 16:
                    tables_b(st + 1)
                scan_back(st)
            P.barrier()
        tci = 0
        tc = [sbt(f"tcg{i}", [128, 512], F32) for i in range(8)]
        wglu = sbt("wglu", [128, 4, 1024], BF16)
        P.dma("pool", wglu[:], k.w_glu[l].rearrange("(k p) c -> p k c", p=128), writes=["wglu"], semkey="wglu")
        yo = [sbt(f"yco{i}", [128, 512], BF16) for i in range(2)]
        it = 0
        for oc in range(4):
            for tq in range(4):
                cs_ = slice(tq * 512, (tq + 1) * 512)
                b = it % 2
                it += 1
                psV, psG = k.ps[b], k.ps[2 + b]
                for kk in range(4):
                    S("pe", lambda e, psV=psV, kk=kk, oc=oc, cs_=cs_: e.matmul(psV[:], wglu[:, kk, oc * 128:(oc + 1) * 128], gel[:, kk, cs_],
                                                                       start=(kk == 0), stop=(kk == 3)),
                      ["wglu", ("gel", kk, tq)], [("ps", b)])
                for kk in range(4):
                    S("pe", lambda e, psG=psG, kk=kk, oc=oc, cs_=cs_: e.matmul(psG[:], wglu[:, kk, 512 + oc * 128:512 + (oc + 1) * 128], gel[:, kk, cs_],
                                                                       start=(kk == 0), stop=(kk == 3)),
                      ["wglu", ("gel", kk, tq)], [("ps", 2 + b)])
                t = tc[tci % 8]
                tk = ("tc", tci % 8)
                tci += 1
                S("act", lambda e, t=t, psG=psG, oc=oc: e.activation(t[:], psG[:], AF.Sigmoid, bias=bglu[:, 4 + oc:5 + oc], scale=1.0),
                  [("ps", 2 + b), "bglu"], [tk])
                S("dve", lambda e, t=t, psV=psV, oc=oc, b=b: e.scalar_tensor_tensor(yo[b][:], psV[:], bglu[:, oc:oc + 1], t[:], ALU.add, ALU.mult),
                  [("ps", b), "bglu", tk], [("yco", b)])
                P.dma("sp", k.s_yc[oc * 128:(oc + 1) * 128, cs_], yo[b][:], reads=[("yco", b)], semkey=("yco", b))
        P.barrier()


class LNBufs:
    def __init__(self, k, es, moe, l):
        nc, P = k.nc, k.P
        sbt = lambda n, s, d: es.enter_context(SBT(nc, n, list(s), d))
        self.stats = sbt("ln_stats", [128, 12], F32)
        self.mv = sbt("ln_mv", [128, 2], F32)
        self.std = sbt("ln_std", [128, 1], F32)
        self.rstd = sbt("ln_rstd", [128, 1], F32)
        self.eps = sbt("ln_eps", [128, 1], F32)
        self.gam = sbt("ln_gam", [128, D], F32)
        self.bet = sbt("ln_bet", [128, D], F32)
        P.op("dve", lambda e: e.memset(self.eps[:], EPS), writes=["ln_eps"])
        self.moe = moe
        if moe:
            j = l // 2
            self.hT32 = sbt("hT32", [128, 8, 128], F32)
            self.rt32 = sbt("rt32", [128, 8, 8], F32)
            self.rbb = sbt("rbb", [128, 8], F32)
            self.lg = sbt("lg", [128, 8], F32)
            self.m8 = sbt("lm8", [128, 8], F32)
            self.sel = sbt("lsel", [128, 8], F32)
            self.nm = sbt("lnm", [128, 1], F32)
            self.ex = sbt("lex", [128, 8], F32)
            self.den = sbt("lden", [128, 1], F32)
            self.gate = sbt("lgate", [128, 8], F32)
            self.gTs = sbt("lgTs", [8, 128], F32)
            P.dma("sp", self.rt32[:], k.moe_router[j].rearrange("(c p) e -> p c e", p=128), writes=["rt32"], semkey="rt32")
            P.dma("sp", self.rbb[:], k.moe_router_b[j:j + 1, :].partition_broadcast(128), writes=["rbb"], semkey="rbb")

    def load_params(self, k, g_ap, b_ap):
        P = k.P
        P.dma("sp", self.gam[:], g_ap.partition_broadcast(128), writes=["ln_gam"], semkey="ln_gam")
        P.dma("sp", self.bet[:], b_ap.partition_broadcast(128), writes=["ln_bet"], semkey="ln_bet")


def ln_tile(k, B, s, skey, tt, dst, route=False, gb_eng="pool", part="all"):
    P = k.P
    S = lambda eng, fn, r, w: P.op(eng, fn, reads=r, writes=w)
    if part in ("all", "math"):
        _ln_math(k, B, s, skey, tt, dst, gb_eng)
    if part in ("all", "tr"):
        _ln_tr(k, B, s, skey, tt, route)


def _ln_math(k, B, s, skey, tt, dst, gb_eng):
    P = k.P
    S = lambda eng, fn, r, w: P.op(eng, fn, reads=r, writes=w)
    S("dve", lambda e: e.bn_stats(B.stats[:, 0:6], s[:, 0:512]), [skey], ["ln_stats"])
    S("dve", lambda e: e.bn_stats(B.stats[:, 6:12], s[:, 512:1024]), [skey], ["ln_stats"])
    S("dve", lambda e: e.bn_aggr(B.mv[:], B.stats[:]), ["ln_stats"], ["ln_mv"])
    S("act", lambda e: e.activation(B.std[:], B.mv[:, 1:2], AF.Sqrt, bias=B.eps[:], scale=1.0), ["ln_mv", "ln_eps"], ["ln_std"])
    S("dve", lambda e: e.reciprocal(B.rstd[:], B.std[:]), ["ln_std"], ["ln_rstd"])
    S("dve", lambda e: e.tensor_scalar(s[:], s[:], B.mv[:, 0:1], B.rstd[:, 0:1], ALU.subtract, ALU.mult), [skey, "ln_mv", "ln_rstd"], [skey])
    S(gb_eng, lambda e: e.tensor_tensor(s[:], s[:], B.gam[:], ALU.mult), [skey, "ln_gam"], [skey])
    S(gb_eng, lambda e: e.tensor_tensor(s[:], s[:], B.bet[:], ALU.add), [skey, "ln_bet"], [skey])
    P.dma("sp", dst, s[:], reads=[skey], semkey=("lnout", skey))


def _ln_tr(k, B, s, skey, tt, route):
    P = k.P
    S = lambda eng, fn, r, w: P.op(eng, fn, reads=r, writes=w)
    for half in range(2):
        ps = k.ps[6 + half]
        pk = ("ps", 6 + half)
        for j in range(4):
            dc = half * 4 + j
            P.op("pe", lambda e, ps=ps, j=j, dc=dc: e.transpose(ps[:, j * 128:(j + 1) * 128], s[:, dc * 128:(dc + 1) * 128], k.ident[:]),
                 reads=[skey, "ident"], writes=[pk], pe_chain=(j > 0))
        P.op("act", lambda e, ps=ps, half=half: e.activation(
            k.hT[:, half * 4:(half + 1) * 4, tt * 128:(tt + 1) * 128],
            ps[:].rearrange("p (j t) -> p j t", j=4), AF.Copy), reads=[pk], writes=[("hT", tt)])
        if route:
            P.op("dve", lambda e, ps=ps, half=half: e.tensor_copy(
                B.hT32[:, half * 4:(half + 1) * 4, :], ps[:].rearrange("p (j t) -> p j t", j=4)), reads=[pk], writes=["hT32"])
    if route:
        psL = k.ps[5]
        for dc in range(8):
            P.op("pe", lambda e, dc=dc: e.matmul(psL[:, 0:8], B.hT32[:, dc, :], B.rt32[:, dc, :], start=(dc == 0), stop=(dc == 7)),
                 reads=["hT32", "rt32"], writes=[("ps", 5)], pe_chain=(dc > 0))
        S("dve", lambda e: e.tensor_tensor(B.lg[:], psL[:, 0:8], B.rbb[:], ALU.add), [("ps", 5), "rbb"], ["lg"])
        S("dve", lambda e: e.max(B.m8[:], B.lg[:]), ["lg"], ["lm8"])
        S("dve", lambda e: e.tensor_scalar(B.sel[:], B.lg[:], B.m8[:, 1:2], None, ALU.is_ge), ["lg", "lm8"], ["lsel"])
        S("dve", lambda e: e.tensor_scalar(B.nm[:], B.m8[:, 0:1], -1.0, None, ALU.mult), ["lm8"], ["lnm"])
        S("act", lambda e: e.activation(B.ex[:], B.lg[:], AF.Exp, bias=B.nm[:], scale=1.0), ["lg", "lnm"], ["lex"])
        S("dve", lambda e: e.tensor_tensor(B.ex[:], B.ex[:], B.sel[:], ALU.mult), ["lex", "lsel"], ["lex"])
        S("dve", lambda e: e.reduce_sum(B.den[:], B.ex[:], AX.X), ["lex"], ["lden"])
        S("dve", lambda e: e.reciprocal(B.den[:], B.den[:]), ["lden"], ["lden"])
        S("dve", lambda e: e.tensor_scalar(B.gate[:], B.ex[:], B.den[:, 0:1], None, ALU.mult), ["lex", "lden"], ["lgate"])
        P.dma("sp", k.s_gate[tt * 128:(tt + 1) * 128, :], B.gate[:], reads=["lgate"], semkey="lgate")


def stage_merge(k, l):
    P, nc = k.P, k.nc
    moe = (l % 2 == 1)
    hsrc = k.x if l == 0 else k.s_h
    with ExitStack() as es:
        sbt = lambda n, s, d: es.enter_context(SBT(nc, n, list(s), d))
        wbr = sbt("wbr", [128, 12, 1024], BF16)
        wout = sbt("wout", [128, 8, 1024], BF16)
        ytq = [sbt(f"ytq{i}", [128, 12, 512], BF16) for i in range(2)]
        gtq2 = [sbt(f"gtq{i}", [128, 24, 512], BF16) for i in range(2)]
        mT = [sbt(f"mT{i}", [128, 8, 512], BF16) for i in range(2)]
        mt = [sbt(f"mtmp{i}", [128, 512], F32) for i in range(4)]
        hti = [sbt(f"hti{i}", [128, D], F32) for i in range(2)]
        st_ = [sbt(f"lns{i}", [128, D], F32) for i in range(2)]
        B = LNBufs(k, es, moe, l)
        B.load_params(k, k.ln1_g[l:l + 1, :], k.ln1_b[l:l + 1, :])
        P.dma("pool", wbr[:], k.w_branch[l].rearrange("(c p) d -> p c d", p=128), writes=["wbr"], semkey="wbr")
        P.dma("pool", wout[:], k.w_out[l].rearrange("(c p) d -> p c d", p=128), writes=["wout"], semkey="wout")
        ysrc = [k.s_ya, k.s_yb, k.s_yc]
        mi = 0
        ti = 0
        pend = [None]
        for tq in range(4):
            cs_ = slice(tq * 512, (tq + 1) * 512)
            yb = tq % 2
            for n in range(3):
                P.dma("sp", ytq[yb][:, n * 4:(n + 1) * 4, :], ysrc[n].rearrange("(c p) t -> p c t", p=128)[:, :, cs_],
                      writes=[("ytq", yb)], semkey=("ytq", yb))
            gtq = gtq2[tq % 2]
            gk = ("gtq", tq % 2)
            P.dma("sp", gtq[:], k.s_g.rearrange("(c p) t -> p c t", p=128)[:, :, cs_], writes=[gk], semkey=gk)
            mb = tq % 2
            for dc in range(8):
                for n in range(3):
                    for kk in range(4):
                        P.op("pe", lambda e, n=n, kk=kk, dc=dc, yb=yb: e.matmul(
                            k.ps[n][:], wbr[:, n * 4 + kk, dc * 128:(dc + 1) * 128], ytq[yb][:, n * 4 + kk, :],
                            start=(kk == 0), stop=(kk == 3)), reads=["wbr", ("ytq", yb)], writes=[("ps", n)], pe_chain=(kk > 0))
                t = [mt[(mi + i) % 4] for i in range(2)]
                tk = [("mtmp", (mi + i) % 4) for i in range(2)]
                mi += 2
                P.op("dve", lambda e, t=t, dc=dc, gtq=gtq: e.tensor_tensor(t[0][:], k.ps[0][:], gtq[:, dc, :], ALU.mult), reads=[("ps", 0), gk], writes=[tk[0]])
                P.op("dve", lambda e, t=t, dc=dc, gtq=gtq: e.tensor_tensor(t[1][:], k.ps[1][:], gtq[:, 8 + dc, :], ALU.mult), reads=[("ps", 1), gk], writes=[tk[1]])
                P.op("dve", lambda e, t=t: e.tensor_tensor(t[0][:], t[0][:], t[1][:], ALU.add), reads=[tk[0], tk[1]], writes=[tk[0]])
                P.op("dve", lambda e, t=t, dc=dc, gtq=gtq: e.tensor_tensor(t[1][:], k.ps[2][:], gtq[:, 16 + dc, :], ALU.mult), reads=[("ps", 2), gk], writes=[tk[1]])
                P.op("dve", lambda e, t=t, dc=dc, mb=mb: e.tensor_tensor(mT[mb][:, dc, :], t[0][:], t[1][:], ALU.add),
                     reads=[tk[0], tk[1]], writes=[("mT", mb)])
            for j in range(4):
                tt = tq * 4 + j
                b = ti % 2
                ti += 1
                P.dma("sp", hti[b][:], hsrc[tt * 128:(tt + 1) * 128, :], writes=[("hti", b)], semkey=("hti", b))
                for half in range(2):
                    psW = k.ps[3 + half]
                    for dc in range(8):
                        P.op("pe", lambda e, psW=psW, dc=dc, j=j, half=half, mb=mb: e.matmul(
                            psW[:], mT[mb][:, dc, j * 128:(j + 1) * 128], wout[:, dc, half * 512:(half + 1) * 512],
                            start=(dc == 0), stop=(dc == 7)), reads=[("mT", mb), "wout"], writes=[("ps", 3 + half)], pe_chain=(dc > 0))
                    P.op("dve", lambda e, psW=psW, b=b, half=half: e.scalar_tensor_tensor(
                        st_[b][:, half * 512:(half + 1) * 512], hti[b][:, half * 512:(half + 1) * 512], ALPHA, psW[:], ALU.mult, ALU.add),
                        reads=[("hti", b), ("ps", 3 + half)], writes=[("lns", b)])
                ln_tile(k, B, st_[b], ("lns", b), tt, k.s_h[tt * 128:(tt + 1) * 128, :], route=moe, gb_eng="dve", part="math")
                if pend[0] is not None:
                    pb_, ptt = pend[0]
                    ln_tile(k, B, st_[pb_], ("lns", pb_), ptt, None, route=moe, gb_eng="dve", part="tr")
                pend[0] = (b, tt)
        pb_, ptt = pend[0]
        ln_tile(k, B, st_[pb_], ("lns", pb_), ptt, None, route=moe, gb_eng="dve", part="tr")
        P.barrier()


def stage_ffn(k, l):
    P, nc = k.P, k.nc
    moe = (l % 2 == 1)
    jl = l // 2
    last = (l == 3)
    ne = 8 if moe else 1
    w1s = k.moe_w1 if moe else k.ffn_w1
    w3s = k.moe_w3 if moe else k.ffn_w3
    w2s = k.moe_w2 if moe else k.ffn_w2
    TH = 1024
    with ExitStack() as es:
        sbt = lambda n, s, d: es.enter_context(SBT(nc, n, list(s), d))
        w2sb = [sbt(f"w2sb{i}", [128, NF, 512], BF16) for i in range(2)]
        actT = sbt("actT", [128, NF, TH], BF16)
        w13 = [sbt(f"w13_{i}", [128, 2, 8, 256], BF16) for i in range(3)]
        sa = [sbt(f"sa{i}", [128, 512], F32) for i in range(3)]
        st_ = [sbt(f"flns{i}", [128, D], F32) for i in range(2)]
        facc = sbt("facc", [128, 8, D], F32)
        B = LNBufs(k, es, False, l)
        B.load_params(k, k.ln2_g[l:l + 1, :], k.ln2_b[l:l + 1, :])
        if moe:
            gsb = sbt("gsb", [128, NT, 8], F32)
            P.dma("sp", gsb[:], k.s_gate.rearrange("(t p) e -> p t e", p=128), writes=["gsb"], semkey="gsb")
        cnt = {"wi": 0, "ai": 0, "wq": 0}

        def up(th, ex, hook=None):
            t0 = th * TH
            ei = jl * 8 + ex if moe else jl
            w1v = w1s[ei].rearrange("(c p) f -> p c f", p=128)
            w3v = w3s[ei].rearrange("(c p) f -> p c f", p=128)
            w2v = w2s[ei].rearrange("(c p) d -> p c d", p=128)
            for fb in range(11):
                if hook is not None:
                    hook(fb)
                if fb == 3:
                    for hd in range(2):
                        P.dma("pool", w2sb[hd][:], w2v[:, :, hd * 512:(hd + 1) * 512], writes=[("w2sb", hd)], semkey=("w2sb", hd))
                wb_ = cnt["wi"] % 3
                cnt["wi"] += 1
                P.dma("pool", w13[wb_][:, 0, :, :], w1v[:, :, fb * 256:(fb + 1) * 256], writes=[("w13", wb_)], semkey=("w13", wb_))
                P.dma("pool", w13[wb_][:, 1, :, :], w3v[:, :, fb * 256:(fb + 1) * 256], writes=[("w13", wb_)], semkey=("w13", wb_))
                for jj in range(2):
                    fj = fb * 2 + jj
                    for c2 in range(2):
                        tcols = slice(t0 + c2 * 512, t0 + (c2 + 1) * 512)
                        pa = cnt["ai"] % 3
                        cnt["ai"] += 1
                        psA, psBm = k.ps[pa * 2], k.ps[pa * 2 + 1]
                        hk = [("hT", t0 // 128 + c2 * 4 + q) for q in range(4)]
                        for kk in range(8):
                            P.op("pe", lambda e, psA=psA, kk=kk, jj=jj, wb_=wb_, tcols=tcols: e.matmul(
                                psA[:], w13[wb_][:, 0, kk, jj * 128:(jj + 1) * 128], k.hT[:, kk, tcols], start=(kk == 0), stop=(kk == 7)),
                                reads=[("w13", wb_)] + hk, writes=[("ps", pa * 2)], pe_chain=(kk > 0))
                        for kk in range(8):
                            P.op("pe", lambda e, psBm=psBm, kk=kk, jj=jj, wb_=wb_, tcols=tcols: e.matmul(
                                psBm[:], w13[wb_][:, 1, kk, jj * 128:(jj + 1) * 128], k.hT[:, kk, tcols], start=(kk == 0), stop=(kk == 7)),
                                reads=[("w13", wb_)] + hk, writes=[("ps", pa * 2 + 1)], pe_chain=(kk > 0))
                        P.op("act", lambda e, psA=psA, pa=pa: e.activation(sa[pa][:], psA[:], AF.Silu), reads=[("ps", pa * 2)], writes=[("sa", pa)])
                        ocols = slice(c2 * 512, (c2 + 1) * 512)
                        P.op("dve", lambda e, psBm=psBm, pa=pa, fj=fj, ocols=ocols: e.tensor_tensor(actT[:, fj, ocols], psBm[:], sa[pa][:], ALU.mult),
                             reads=[("ps", pa * 2 + 1), ("sa", pa)], writes=[("actT", fj, c2)])

        def down(th, ex, hook=None):
            t0 = th * TH
            ei = jl * 8 + ex if moe else jl
            for tl in range(8):
                for hd in range(2):
                    tt = t0 // 128 + tl
                    psW = k.ps[6 + (cnt["wq"] % 2)]
                    pwk = ("ps", 6 + (cnt["wq"] % 2))
                    cnt["wq"] += 1
                    ak = [("actT", fj, tl // 4) for fj in range(NF)]
                    for fj in range(NF):
                        P.op("pe", lambda e, psW=psW, fj=fj, tl=tl, hd=hd: e.matmul(
                            psW[:], actT[:, fj, tl * 128:(tl + 1) * 128], w2sb[hd][:, fj, :], start=(fj == 0), stop=(fj == NF - 1)),
                            reads=[("w2sb", hd)] + (ak if fj == 0 else []), writes=[pwk], pe_chain=(fj > 0))
                    fk = ("facc", tl, hd)
                    fsl = facc[:, tl, hd * 512:(hd + 1) * 512]
                    if moe:
                        gap = gsb[:, tt, ex:ex + 1]
                        if ex == 0:
                            P.op("dve", lambda e, psW=psW, fsl=fsl, gap=gap: e.tensor_scalar(fsl, psW[:], gap, None, ALU.mult),
                                 reads=[pwk, "gsb"], writes=[fk])
                        else:
                            P.op("dve", lambda e, psW=psW, fsl=fsl, gap=gap: e.scalar_tensor_tensor(fsl, psW[:], gap, fsl, ALU.mult, ALU.add),
                                 reads=[pwk, "gsb", fk], writes=[fk])
                    else:
                        P.op("act", lambda e, psW=psW, fsl=fsl: e.activation(fsl, psW[:], AF.Copy), reads=[pwk], writes=[fk])
                if hook is not None:
                    hook(tl)

        def ln_part(th, tl, part):
            t0 = th * TH
            tt = t0 // 128 + tl
            b = tl % 2
            sk = ("flns", b)
            dst = (k.y if last else k.s_h)[tt * 128:(tt + 1) * 128, :]
            if part == "math":
                P.dma("sp", st_[b][:], k.s_h[tt * 128:(tt + 1) * 128, :], writes=[sk], semkey=("fhti", b))
                P.op("dve", lambda e: e.scalar_tensor_tensor(st_[b][:], st_[b][:], ALPHA, facc[:, tl, :], ALU.mult, ALU.add),
                     reads=[sk, ("facc", tl, 0), ("facc", tl, 1)], writes=[sk])
            ln_tile(k, B, st_[b], sk, tt, dst, route=False, gb_eng="dve", part=part)

        def hook0(fb):
            if 1 <= fb <= 8:
                ln_part(0, fb - 1, "math")
            if 2 <= fb <= 9:
                ln_part(0, fb - 2, "tr")

        for ex in range(ne):
            up(0, ex)
            down(0, ex)
        def hook1(tl):
            ln_part(1, tl, "math")
            if tl >= 1:
                ln_part(1, tl - 1, "tr")

        up(1, 0, hook0)
        down(1, 0, hook1 if ne == 1 else None)
        for ex in range(1, ne):
            up(1, ex)
            down(1, ex, hook1 if ex == ne - 1 else None)
        ln_part(1, 7, "tr")
        P.barrier()


def kernel(**inputs):
    inp = {k_: np.asarray(v) for k_, v in inputs.items()}
    lay = host_layout(inp)
    cst = host_consts()
    nc = build_program(4)
    base = dict(lay)
    base.update(cst)
    in_maps = []
    for b in range(8):
        m = dict(base)
        m["x"] = np.ascontiguousarray(inp["x"][b])
        in_maps.append(m)
    res = run_bass_kernel_spmd(nc, in_maps, core_ids=list(range(8)))
    out = np.stack([np.asarray(r["y"]) for r in res.results], axis=0).astype(np.float32)
    return out
```
